# Optimizing a Trainium2 kernel written in Bass

```python
import jax, jax.numpy as jnp
from jax import lax
import numpy as np

D_MODEL = 4096
BATCH = 4
SEQ = 4096
DEPTH = 1

PLE_DIM = 256
HEAD_DIM = 128
MOBA_HEADS = D_MODEL // (2 * HEAD_DIM)
SB_HEADS = D_MODEL // (2 * HEAD_DIM)
MOBA_WIDTH = MOBA_HEADS * HEAD_DIM
SB_WIDTH = SB_HEADS * HEAD_DIM
MOBA_BLOCK = 256
MOBA_TOPK = 3
MOBA_Q_CHUNK = 16
SB_Q_BLOCK = 128
D_FF = ((8 * D_MODEL // 3 + 255) // 256) * 256
RMS_EPS = 1e-6
IN_SPLITS = [MOBA_WIDTH, MOBA_WIDTH, MOBA_WIDTH, SB_WIDTH, SB_WIDTH, SB_WIDTH, D_MODEL, D_MODEL]
IN_COLS = sum(IN_SPLITS)

kernel_name = "hybrid_moba_stickbreaking_gated_block"


def rms_norm(x, g):
    xf = x.astype(jnp.float32)
    xf = xf * lax.rsqrt(jnp.mean(xf * xf, axis=-1, keepdims=True) + RMS_EPS)
    return xf.astype(x.dtype) * g


def swiglu(x, w_gate, w_up, w_down):
    return (jax.nn.silu(x @ w_gate) * (x @ w_up)) @ w_down


def alibi_slopes(n_heads):
    return jnp.asarray(2.0 ** (-8.0 * np.arange(1, n_heads + 1) / n_heads), dtype=jnp.float32)


def split_heads(t, n_heads):
    b, s, _ = t.shape
    return t.reshape(b, s, n_heads, HEAD_DIM).transpose(0, 2, 1, 3)


def merge_heads(t):
    b, h, s, d = t.shape
    return t.transpose(0, 2, 1, 3).reshape(b, s, h * d)


def moba_attention(q, k, v):
    B, H, S, dh = q.shape
    L = MOBA_BLOCK
    nb = -(-S // L)
    pad = nb * L - S
    k_pad = jnp.pad(k, ((0, 0), (0, 0), (0, pad), (0, 0)))
    v_pad = jnp.pad(v, ((0, 0), (0, 0), (0, pad), (0, 0)))
    kb = k_pad.reshape(B, H, nb, L, dh)
    vb = v_pad.reshape(B, H, nb, L, dh)
    counts = np.minimum(L, S - np.arange(nb) * L).astype(np.float32)
    k_mean = kb.astype(jnp.float32).sum(axis=3) / jnp.asarray(counts)[:, None]
    gate = jnp.einsum('bhsd,bhnd->bhsn', q.astype(jnp.float32), k_mean)
    q_block = jnp.arange(S) // L
    fully_past = jnp.arange(nb)[None, :] < q_block[:, None]
    gate = jnp.where(fully_past, gate, -jnp.inf)
    n_sel = min(MOBA_TOPK, nb)
    _, sel = lax.top_k(gate, n_sel)
    sel_ok = sel < q_block[:, None]

    scale = dh ** -0.5
    slopes = alibi_slopes(H)
    C = MOBA_Q_CHUNK
    bi = jnp.arange(B)[:, None, None, None]
    hi = jnp.arange(H)[None, :, None, None]

    def chunk(c):
        t0 = c * C
        qc = lax.dynamic_slice_in_dim(q, t0, C, axis=2)
        sc = lax.dynamic_slice_in_dim(sel, t0, C, axis=2)
        okc = lax.dynamic_slice_in_dim(sel_ok, t0, C, axis=2)
        t = (t0 + jnp.arange(C)).astype(jnp.float32)
        k_sel = kb[bi, hi, sc]
        v_sel = vb[bi, hi, sc]
        s_sel = (sc[..., None] * L + jnp.arange(L)).astype(jnp.float32)
        logit_sel = (jnp.einsum('bhcd,bhcnld->bhcnl', qc, k_sel).astype(jnp.float32) * scale
                     - slopes[:, None, None, None] * jnp.abs(t[:, None, None] - s_sel))
        logit_sel = jnp.where(okc[..., None], logit_sel, -jnp.inf)
        b0 = t0 // L
        k_own = lax.dynamic_slice_in_dim(k_pad, b0 * L, L, axis=2)
        v_own = lax.dynamic_slice_in_dim(v_pad, b0 * L, L, axis=2)
        s_own = (b0 * L + jnp.arange(L)).astype(jnp.float32)
        logit_own = (jnp.einsum('bhcd,bhld->bhcl', qc, k_own).astype(jnp.float32) * scale
                     - slopes[:, None, None] * jnp.abs(t[:, None] - s_own[None, :]))
        logit_own = jnp.where(s_own[None, :] <= t[:, None], logit_own, -jnp.inf)
        logits = jnp.concatenate([logit_sel.reshape(B, H, C, n_sel * L), logit_own], axis=-1)
        probs = jax.nn.softmax(logits, axis=-1)
        p_sel = probs[..., :n_sel * L].reshape(B, H, C, n_sel, L).astype(v.dtype)
        p_own = probs[..., n_sel * L:].astype(v.dtype)
        return (jnp.einsum('bhcnl,bhcnld->bhcd', p_sel, v_sel)
                + jnp.einsum('bhcl,bhld->bhcd', p_own, v_own))

    outs = lax.map(chunk, jnp.arange(S // C))
    return outs.transpose(1, 2, 0, 3, 4).reshape(B, H, S, dh)


def stick_breaking_attention(q, k, v):
    B, H, S, dh = q.shape
    scale = dh ** -0.5
    outs = []
    for i in range(S // SB_Q_BLOCK):
        t0, t1 = i * SB_Q_BLOCK, (i + 1) * SB_Q_BLOCK
        z = jnp.einsum('bhqd,bhsd->bhqs', q[:, :, t0:t1], k[:, :, :t1]).astype(jnp.float32) * scale
        t = jnp.arange(t0, t1)
        s = jnp.arange(t1)
        strict = s[None, :] < t[:, None]
        log_keep = jnp.where(strict, jax.nn.log_sigmoid(-z), 0.0)
        later = lax.cumsum(log_keep, axis=3, reverse=True) - log_keep
        w = jnp.where(strict, jnp.exp(jax.nn.log_sigmoid(z) + later), 0.0)
        outs.append(jnp.einsum('bhqs,bhsd->bhqd', w.astype(v.dtype), v[:, :, :t1]))
    return jnp.concatenate(outs, axis=2)


def setup_inputs(seed: int = 0) -> dict:
    key = jax.random.key(seed)
    ks = jax.random.split(key, 24)
    f32 = jnp.float32

    def w(k, shape, fan_in):
        return jax.random.normal(k, shape, f32) * (fan_in ** -0.5)

    def gain(k, shape):
        return 1.0 + 0.02 * jax.random.normal(k, shape, f32)

    return {
        "x": jax.random.normal(ks[0], (BATCH, SEQ, D_MODEL), f32),
        "p": jax.random.normal(ks[1], (DEPTH, BATCH, SEQ, PLE_DIM), f32),
        "ffn1_norm": gain(ks[2], (DEPTH, D_MODEL)),
        "ffn1_w_gate": w(ks[3], (DEPTH, D_MODEL, D_FF), D_MODEL),
        "ffn1_w_up": w(ks[4], (DEPTH, D_MODEL, D_FF), D_MODEL),
        "ffn1_w_down": w(ks[5], (DEPTH, D_FF, D_MODEL), D_FF),
        "mix_norm": gain(ks[6], (DEPTH, D_MODEL)),
        "w_in": w(ks[7], (DEPTH, D_MODEL, IN_COLS), D_MODEL),
        "w_branch_moba": w(ks[8], (DEPTH, MOBA_WIDTH, D_MODEL), MOBA_WIDTH),
        "w_branch_sb": w(ks[9], (DEPTH, SB_WIDTH, D_MODEL), SB_WIDTH),
        "w_out": w(ks[10], (DEPTH, D_MODEL, D_MODEL), D_MODEL),
        "ffn2_norm": gain(ks[11], (DEPTH, D_MODEL)),
        "ffn2_w_gate": w(ks[12], (DEPTH, D_MODEL, D_FF), D_MODEL),
        "ffn2_w_up": w(ks[13], (DEPTH, D_MODEL, D_FF), D_MODEL),
        "ffn2_w_down": w(ks[14], (DEPTH, D_FF, D_MODEL), D_FF),
        "ple_norm": gain(ks[15], (DEPTH, D_MODEL)),
        "w_ple_gate": w(ks[16], (DEPTH, D_MODEL, D_MODEL), D_MODEL),
        "w_ple_proj": w(ks[17], (DEPTH, PLE_DIM, D_MODEL), PLE_DIM),
        "final_norm": gain(ks[18], (D_MODEL,)),
    }


def reference(x, p, ffn1_norm, ffn1_w_gate, ffn1_w_up, ffn1_w_down, mix_norm, w_in,
              w_branch_moba, w_branch_sb, w_out, ffn2_norm, ffn2_w_gate, ffn2_w_up,
              ffn2_w_down, ple_norm, w_ple_gate, w_ple_proj, final_norm):
    h = x
    offsets = [int(o) for o in np.cumsum(IN_SPLITS)[:-1]]
    for i in range(DEPTH):
        h = h + 0.5 * swiglu(rms_norm(h, ffn1_norm[i]), ffn1_w_gate[i], ffn1_w_up[i], ffn1_w_down[i])
        u = rms_norm(h, mix_norm[i])
        qa, ka, va, qb, kb, vb, ga, gb = jnp.split(u @ w_in[i], offsets, axis=-1)
        y_a = merge_heads(moba_attention(split_heads(qa, MOBA_HEADS), split_heads(ka, MOBA_HEADS),
                                         split_heads(va, MOBA_HEADS))) @ w_branch_moba[i]
        y_b = merge_heads(stick_breaking_attention(split_heads(qb, SB_HEADS), split_heads(kb, SB_HEADS),
                                                   split_heads(vb, SB_HEADS))) @ w_branch_sb[i]
        h = h + (jax.nn.sigmoid(ga) * y_a + jax.nn.sigmoid(gb) * y_b) @ w_out[i]
        h = h + 0.5 * swiglu(rms_norm(h, ffn2_norm[i]), ffn2_w_gate[i], ffn2_w_up[i], ffn2_w_down[i])
        h = h + jax.nn.sigmoid(rms_norm(h, ple_norm[i]) @ w_ple_gate[i]) * (p[i] @ w_ple_proj[i])
    return rms_norm(h, final_norm)
```

```python
import numpy as np
import ml_dtypes
from contextlib import ExitStack
import concourse.bass as bass
import concourse.mybir as mybir
from concourse.bass_utils import run_bass_kernel_spmd

F32 = mybir.dt.float32
BF16 = mybir.dt.bfloat16
AF = mybir.ActivationFunctionType
ALU = mybir.AluOpType
AX = mybir.AxisListType

RMS_EPS = 1e-6
BIG = 30000.0
NDS = 24
WK = 16


class Cfg:
    def __init__(self, D, F, SH, NT, H, B):
        self.D, self.F, self.SH, self.NT, self.H, self.B = D, F, SH, NT, H, B
        self.KC = D // 128
        self.FC = F // 128
        self.TT = SH // NT
        self.SV = 2 * SH
        self.QT = SH // 128
        self.NBLK = self.SV // 256
        self.NQKV = 6 * H
        self.INC = 6 * H * 128 + 2 * D
        self.C0 = 256 * (self.QT - 1)
        self.TABW = 256 * self.QT
        self.NP = (4 * H) // 4
        self.GC = min(32, self.FC)
        self.NCORES = 2 * B


FULL = Cfg(D=4096, F=11008, SH=2048, NT=512, H=16, B=4)


class Buf:
    __slots__ = ("name", "w", "r")

    def __init__(self, name):
        self.name = name
        self.w = None
        self.r = {}


class Sched:
    def __init__(self, nc, es):
        self.nc = nc
        self.eng = {"pe": nc.tensor, "act": nc.scalar, "dve": nc.vector, "pool": nc.gpsimd, "sp": nc.sync}
        self.sem = {k: es.enter_context(nc.semaphore("s_" + k)) for k in self.eng}
        self.cnt = {k: 0 for k in self.eng}
        self.dsem = [es.enter_context(nc.semaphore("d%d" % i)) for i in range(NDS)]
        self.dcnt = [0] * NDS
        self.dnext = {"sp": 0, "pool": 0, "act": 0}
        self.dbase = {"sp": 0, "pool": NDS // 2, "act": 0}
        self.ops = {k: [] for k in self.eng}
        self.waited = {k: {} for k in self.eng}
        self.csem = []
        self.es = es

    def op(self, e, fn, reads=(), writes=(), dma=False):
        deps = []
        for b in reads:
            if b.w is not None:
                deps.append(b.w)
        for b in writes:
            if b.w is not None:
                deps.append(b.w)
            for k, v in b.r.items():
                deps.append((k, v))
        if dma:
            i = self.dbase[e] + self.dnext[e]
            self.dnext[e] = (self.dnext[e] + 1) % (NDS // 2)
            if self.dcnt[i] > 0:
                deps.append((("d", i), self.dcnt[i]))
            self.dcnt[i] += 16
            ev = (("d", i), self.dcnt[i])
        else:
            self.cnt[e] += 1
            ev = (("e", e), self.cnt[e])
        waits = {}
        wd = self.waited[e]
        for key, val in deps:
            if key == ("e", "pe") and e == "pe" and not dma:
                continue
            if wd.get(key, 0) >= val:
                continue
            if waits.get(key, 0) < val:
                waits[key] = val
        for k, v in waits.items():
            wd[k] = v
        self.ops[e].append((list(waits.items()), fn, ev))
        for b in reads:
            if b.r.get(ev[0], 0) < ev[1]:
                b.r[ev[0]] = ev[1]
        for b in writes:
            b.w = ev
            b.r = {}
        return ev

    def cc_op(self, fn, writes):
        i = len(self.csem)
        self.csem.append(self.es.enter_context(self.nc.semaphore("cc%d" % i)))
        ev = (("c", i), 1)
        self.ops["pool"].append(([], fn, ev))
        for b in writes:
            b.w = ev
            b.r = {}
        return ev

    def drain_dmas(self, e="sp"):
        waits = []
        for i in range(NDS):
            if self.dcnt[i] > self.waited[e].get(("d", i), 0):
                waits.append((("d", i), self.dcnt[i]))
                self.waited[e][("d", i)] = self.dcnt[i]
        self.ops[e].append((waits, None, None))

    def _semof(self, key):
        if key[0] == "c":
            return self.csem[key[1]]
        return self.dsem[key[1]] if key[0] == "d" else self.sem[key[1]]

    def emit(self, block):
        def run(name):
            def body(engine):
                for waits, fn, ev in self.ops[name]:
                    for key, val in waits:
                        engine.wait_ge(self._semof(key), val)
                    if fn is None:
                        continue
                    inst = fn(engine)
                    if ev[0][0] == "c":
                        inst.then_inc(self._semof(ev[0]))
                    else:
                        inst.then_inc(self._semof(ev[0]), 16 if ev[0][0] == "d" else 1)
            return body
        for name, dec in (("sp", block.sync), ("pe", block.tensor), ("act", block.scalar),
                          ("dve", block.vector), ("pool", block.gpsimd)):
            if self.ops[name]:
                dec(run(name))
            self.ops[name] = []


def alibi_slope(h, H):
    return float(2.0 ** (-8.0 * (h + 1) / H))


def build_program(cfg):
    D, F, SH, NT, H = cfg.D, cfg.F, cfg.SH, cfg.NT, cfg.H
    KC, FC, TT, SV, QT, NBLK = cfg.KC, cfg.FC, cfg.TT, cfg.SV, cfg.QT, cfg.NBLK
    C0, TABW, GC = cfg.C0, cfg.TABW, cfg.GC
    scale = 128.0 ** -0.5

    nc = bass.Bass("TRN2", target_bir_lowering=False)

    def din(name, shape, dt=F32):
        return nc.dram_tensor(name, list(shape), dt, kind="ExternalInput").ap()

    xT = din("xT", [128, KC, SH])
    pT = din("pT", [128, 2, SH])
    gall = din("gall", [128, 5, KC])
    mbias = din("mbias", [128, QT, NBLK])
    cid = din("cident", [128, 128])
    cms = din("cmask2", [128, 256])
    cb0 = din("cb0m", [128, TABW])
    cind = din("cind", [NBLK, SV])
    Wd_ = {}
    for nm, K, N in (("f1g", D, F), ("f1u", D, F), ("f1d", F, D), ("win", D, cfg.INC), ("wbm", H * 128, D),
                     ("wbs", H * 128, D), ("wout", D, D), ("f2g", D, F), ("f2u", D, F), ("f2d", F, D),
                     ("wpg", D, D), ("wpp", 256, D)):
        Wd_[nm] = din(nm, [N // 128, 128, K // 128, 128])
    outT = nc.dram_tensor("outT", [128, KC, SH], F32, kind="ExternalOutput").ap()
    h1 = nc.dram_tensor("h1s", [128, KC, SH], F32, kind="Internal").ap()
    qTs = nc.dram_tensor("qTs", [2 * H, 128, SH], BF16, kind="Internal").ap()
    kvloc_t = nc.dram_tensor("kvloc", [4 * H * 128, SH], BF16)
    kvloc = kvloc_t.ap()
    kvall_t = [nc.dram_tensor("kvall%d" % p_, [2 * 512, SH], BF16) for p_ in range(cfg.NP)]
    kvall = [t_.ap() for t_ in kvall_t]
    groups = [[2 * b_, 2 * b_ + 1] for b_ in range(cfg.B)]

    def kv_slot(j):
        if 4 * H <= j < 5 * H:
            return 2 * (j - 4 * H)
        if 5 * H <= j < 6 * H:
            return 2 * (j - 5 * H) + 1
        if H <= j < 2 * H:
            return 2 * (H + j - H)
        return 2 * (H + j - 2 * H) + 1
    attnT = nc.dram_tensor("attns", [2 * H, 128, SH], BF16, kind="Internal").ap()

    with ExitStack() as es0:
        S = Sched(nc, es0)
        cnt = [0]

        def sb(es, shape, dt, nm=None):
            cnt[0] += 1
            return es.enter_context(nc.sbuf_tensor("%s_%d" % (nm or "t", cnt[0]), list(shape), dt))

        def ps(es, shape, dt, nm=None):
            cnt[0] += 1
            return es.enter_context(nc.psum_tensor("%s_%d" % (nm or "p", cnt[0]), list(shape), dt))

        def phase_ac(which):
            with ExitStack() as es:
                h = sb(es, [128, KC, NT], F32, "h")
                xn = sb(es, [128, KC, NT], BF16, "xn")
                big2 = sb(es, [128, max(GC, 2 * H), NT], BF16, "big2")
                gated = sb(es, [128, KC, NT], BF16, "gated") if which == "C" else None
                NWS = 6
                wring = [sb(es, [128, WK, 128], BF16, "w") for _ in range(NWS)]
                gsb = sb(es, [128, 5, KC], F32, "g")
                ones32 = sb(es, [128, 128], F32, "ones")
                sq = [sb(es, [128, NT], F32, "sq") for _ in range(2)]
                rs = sb(es, [128, NT], F32, "rs")
                rs2 = sb(es, [128, NT], F32, "rs2")
                tmp = [sb(es, [128, NT], F32, "tmp") for _ in range(4)]
                stage = [sb(es, [128, NT], BF16, "st") for _ in range(4)]
                pbf = sb(es, [128, 2, NT], BF16, "pbf")
                banksF = [ps(es, [128, 512], F32, "bk") for _ in range(8)]
                banks = [b[:, 0:NT] for b in banksF]

                hb = [Buf("h%d" % c) for c in range(KC)]
                xnb = [Buf("xn%d" % c) for c in range(KC)]
                b2b = [Buf("b2_%d" % c) for c in range(max(GC, 2 * H))]
                gtb = [Buf("gt%d" % c) for c in range(KC)]
                wrb = [Buf("w%d" % i) for i in range(NWS)]
                sqb = [Buf("sq0"), Buf("sq1")]
                rsb, rs2b, gb, onesb, pbfb = Buf("rs"), Buf("rs2"), Buf("g"), Buf("ones"), Buf("pbf")
                tmpb = [Buf("tmp%d" % i) for i in range(4)]
                stb = [Buf("st%d" % i) for i in range(4)]
                bkb = [Buf("bk%d" % i) for i in range(8)]
                st = {"w": 0, "tmp": 0, "st": 0, "bk": {}}

                S.op("sp", lambda e: e.dma_start(out=gsb[:], in_=gall), writes=[gb], dma=True)
                S.op("dve", lambda e: e.memset(ones32[:], 1.0), writes=[onesb])

                def mm_group(bank_i, Wt, j, k0, k1, rhs_fn, rhs_bufs):
                    k = k0
                    while k < k1:
                        kk = min(k + WK, k1)
                        si = st["w"] % NWS
                        st["w"] += 1
                        wt, wb = wring[si], wrb[si]
                        S.op("pool", (lambda e, wt=wt, k=k, kk=kk: e.dma_start(
                            out=wt[:, 0:kk - k, :], in_=Wt[j, :, k:kk, :])), writes=[wb], dma=True)

                        def mm(e, wt=wt, k=k, kk=kk):
                            ins = None
                            for q in range(k, kk):
                                ins = e.matmul(banks[bank_i], lhsT=wt[:, q - k, :], rhs=rhs_fn(q),
                                               start=(q == k0), stop=(q == k1 - 1))
                            return ins
                        S.op("pe", mm, reads=[wb] + [rhs_bufs[q] for q in range(k, kk)], writes=[bkb[bank_i]])
                        k = kk

                def nxt(kind, n):
                    i = st[kind] % n
                    st[kind] += 1
                    return i

                def norm(gidx, out_t, out_b):
                    NB = 6
                    for c in range(KC):
                        s = c % 2
                        S.op("act", (lambda e, c=c, s=s: e.activation(out=sq[s][:], in_=h[:, c, :], func=AF.Square)),
                             reads=[hb[c]], writes=[sqb[s]])
                        S.op("pe", (lambda e, c=c, s=s: e.matmul(banks[NB], lhsT=ones32[:], rhs=sq[s][:],
                                                                start=(c == 0), stop=(c == KC - 1))),
                             reads=[sqb[s], onesb], writes=[bkb[NB]])
                    S.op("act", lambda e: e.activation(out=rs[:], in_=banks[NB], func=AF.Sqrt,
                                                       bias=RMS_EPS, scale=1.0 / D),
                         reads=[bkb[NB]], writes=[rsb])
                    S.op("dve", lambda e: e.reciprocal(out=rs2[:], in_=rs[:]), reads=[rsb], writes=[rs2b])
                    for c in range(KC):
                        S.op("dve", (lambda e, c=c: e.scalar_tensor_tensor(
                            out=out_t[:, c, :], in0=h[:, c, :], scalar=gsb[:, gidx, c:c + 1], in1=rs2[:],
                            op0=ALU.mult, op1=ALU.mult)),
                             reads=[hb[c], gb, rs2b], writes=[out_b[c]])

                def ffn(wg, wu, wdn, gidx):
                    norm(gidx, xn, xnb)
                    f0 = 0
                    ng = -(-FC // GC)
                    gs = -(-FC // ng)
                    while f0 < FC:
                        f1 = min(FC, f0 + gs)
                        for f in range(f0, f1):
                            bg = (f % 2)
                            bu = 2 + (f % 2)
                            mm_group(bg, wg, f, 0, KC, lambda q: xn[:, q, :], xnb)
                            mm_group(bu, wu, f, 0, KC, lambda q: xn[:, q, :], xnb)
                            ti = nxt("tmp", 4)
                            S.op("act", (lambda e, bg=bg, ti=ti: e.activation(out=tmp[ti][:], in_=banks[bg],
                                                                            func=AF.Silu)),
                                 reads=[bkb[bg]], writes=[tmpb[ti]])
                            S.op("dve", (lambda e, bu=bu, ti=ti, f=f, f0=f0: e.tensor_tensor(
                                out=big2[:, f - f0, :], in0=banks[bu], in1=tmp[ti][:], op=ALU.mult)),
                                 reads=[bkb[bu], tmpb[ti]], writes=[b2b[f - f0]])
                        for c in range(KC):
                            bd = 4 + (c % 2)
                            mm_group(bd, wdn, c, f0, f1, (lambda q, f0=f0: big2[:, q - f0, :]),
                                     {q: b2b[q - f0] for q in range(f0, f1)})
                            S.op("dve", (lambda e, bd=bd, c=c: e.scalar_tensor_tensor(
                                out=h[:, c, :], in0=banks[bd], scalar=0.5, in1=h[:, c, :],
                                op0=ALU.mult, op1=ALU.add)),
                                 reads=[bkb[bd], hb[c]], writes=[hb[c]])
                        f0 = f1

                if which == "A":
                    for vt in range(TT):
                        own = True
                        t0 = vt * NT
                        S.op("sp", (lambda e, t0=t0: e.dma_start(out=h[:], in_=xT[:, :, t0:t0 + NT])),
                             writes=hb, dma=True)
                        ffn(Wd_["f1g"], Wd_["f1u"], Wd_["f1d"], 0)
                        if own:
                            S.op("sp", (lambda e, t0=t0: e.dma_start(out=h1[:, :, t0:t0 + NT], in_=h[:])),
                                 reads=hb, dma=True)
                        norm(1, xn, xnb)
                        chunks = list(range(6 * H)) if own else (list(range(H, 3 * H)) + list(range(4 * H, 6 * H)))
                        for n_, j in enumerate(chunks):
                            bk = n_ % 4
                            mm_group(bk, Wd_["win"], j, 0, KC, lambda q: xn[:, q, :], xnb)
                            si = nxt("st", 4)
                            if n_ % 2 == 0:
                                S.op("act", (lambda e, bk=bk, si=si: e.activation(out=stage[si][:], in_=banks[bk],
                                                                                func=AF.Copy)),
                                     reads=[bkb[bk]], writes=[stb[si]])
                            else:
                                S.op("dve", (lambda e, bk=bk, si=si: e.tensor_copy(out=stage[si][:], in_=banks[bk])),
                                     reads=[bkb[bk]], writes=[stb[si]])
                            if j < H or 3 * H <= j < 4 * H:
                                dst = qTs[j if j < H else j - 2 * H, :, t0:t0 + NT]
                            else:
                                sl = kv_slot(j)
                                dst = kvloc[sl * 128:(sl + 1) * 128, t0:t0 + NT]
                            S.op("sp", (lambda e, dst=dst, si=si: e.dma_start(out=dst, in_=stage[si][:])),
                                 reads=[stb[si]], dma=True)
                else:
                    for t in range(TT):
                        t0 = t * NT
                        S.op("sp", (lambda e, t0=t0: e.dma_start(out=h[:], in_=h1[:, :, t0:t0 + NT])),
                             writes=hb, dma=True)
                        S.op("sp", (lambda e, t0=t0: e.dma_start(
                            out=big2[:, 0:2 * H, :], in_=attnT[:, :, t0:t0 + NT].rearrange("j p t -> p j t"))),
                             writes=b2b[0:2 * H], dma=True)
                        S.op("pool", (lambda e, t0=t0: e.dma_start(out=pbf[:], in_=pT[:, :, t0:t0 + NT])),
                             writes=[pbfb], dma=True)
                        norm(1, xn, xnb)
                        for j in range(KC):
                            mm_group(0, Wd_["win"], 6 * H + j, 0, KC, lambda q: xn[:, q, :], xnb)
                            mm_group(1, Wd_["win"], 6 * H + KC + j, 0, KC, lambda q: xn[:, q, :], xnb)
                            mm_group(2, Wd_["wbm"], j, 0, H, lambda q: big2[:, q, :], b2b)
                            mm_group(3, Wd_["wbs"], j, 0, H, lambda q: big2[:, H + q, :],
                                     {q: b2b[H + q] for q in range(H)})
                            ta, tb = nxt("tmp", 4), nxt("tmp", 4)
                            S.op("act", (lambda e, ta=ta: e.activation(out=tmp[ta][:], in_=banks[0], func=AF.Sigmoid)),
                                 reads=[bkb[0]], writes=[tmpb[ta]])
                            S.op("act", (lambda e, tb=tb: e.activation(out=tmp[tb][:], in_=banks[1], func=AF.Sigmoid)),
                                 reads=[bkb[1]], writes=[tmpb[tb]])
                            S.op("dve", (lambda e, ta=ta: e.tensor_tensor(out=tmp[ta][:], in0=banks[2], in1=tmp[ta][:],
                                                                         op=ALU.mult)),
                                 reads=[bkb[2], tmpb[ta]], writes=[tmpb[ta]])
                            S.op("dve", (lambda e, tb=tb: e.tensor_tensor(out=tmp[tb][:], in0=banks[3], in1=tmp[tb][:],
                                                                         op=ALU.mult)),
                                 reads=[bkb[3], tmpb[tb]], writes=[tmpb[tb]])
                            S.op("dve", (lambda e, ta=ta, tb=tb, j=j: e.tensor_tensor(
                                out=gated[:, j, :], in0=tmp[ta][:], in1=tmp[tb][:], op=ALU.add)),
                                 reads=[tmpb[ta], tmpb[tb]], writes=[gtb[j]])
                        for c in range(KC):
                            bd = 4 + (c % 2)
                            mm_group(bd, Wd_["wout"], c, 0, KC, lambda q: gated[:, q, :], gtb)
                            S.op("dve", (lambda e, bd=bd, c=c: e.tensor_tensor(out=h[:, c, :], in0=banks[bd],
                                                                             in1=h[:, c, :], op=ALU.add)),
                                 reads=[bkb[bd], hb[c]], writes=[hb[c]])
                        ffn(Wd_["f2g"], Wd_["f2u"], Wd_["f2d"], 2)
                        norm(3, xn, xnb)
                        for c in range(KC):
                            bg, bu = (c % 2), 2 + (c % 2)
                            mm_group(bg, Wd_["wpg"], c, 0, KC, lambda q: xn[:, q, :], xnb)
                            mm_group(bu, Wd_["wpp"], c, 0, 2, lambda q: pbf[:, q, :], {0: pbfb, 1: pbfb})
                            ti = nxt("tmp", 4)
                            S.op("act", (lambda e, bg=bg, ti=ti: e.activation(out=tmp[ti][:], in_=banks[bg],
                                                                            func=AF.Sigmoid)),
                                 reads=[bkb[bg]], writes=[tmpb[ti]])
                            S.op("dve", (lambda e, bu=bu, ti=ti: e.tensor_tensor(out=tmp[ti][:], in0=banks[bu],
                                                                               in1=tmp[ti][:], op=ALU.mult)),
                                 reads=[bkb[bu], tmpb[ti]], writes=[tmpb[ti]])
                            S.op("dve", (lambda e, ti=ti, c=c: e.tensor_tensor(out=h[:, c, :], in0=h[:, c, :],
                                                                              in1=tmp[ti][:], op=ALU.add)),
                                 reads=[tmpb[ti], hb[c]], writes=[hb[c]])
                        norm(4, h, hb)
                        S.op("sp", (lambda e, t0=t0: e.dma_start(out=outT[:, :, t0:t0 + NT], in_=h[:])),
                             reads=hb, dma=True)
                S.drain_dmas("sp")
                with nc.Block() as block:
                    S.emit(block)

        NBUF = 3

        def phase_b(kind, first):
            with ExitStack() as es:
                is_sb = kind == "sb"
                ident = sb(es, [128, 128], BF16, "ident")
                qT2 = [sb(es, [128, SH], BF16, "qT") for _ in range(2)]
                kT2 = [sb(es, [128, SV], BF16, "kT") for _ in range(2)]
                vT = sb(es, [128, SV], BF16, "vT")
                V2 = [sb(es, [128, SV // 128, 128], BF16, "V") for _ in range(2)]
                ah = [sb(es, [128, SH], BF16, "ah") for _ in range(2)]
                NE = 3
                NW, NWT, NSM, NLP = 3, 2, 6, 3
                E = [sb(es, [128, SV], F32, "E") for _ in range(NE)]
                W = [sb(es, [128, SV], BF16, "W") for _ in range(NW)]
                WT = [sb(es, [128, SV // 128, 128], BF16, "WT") for _ in range(NWT)]
                sm = [sb(es, [128, 64], F32, "sm") for _ in range(NSM)]
                if is_sb:
                    negm = sb(es, [128, 256], BF16, "negm")
                    ones_bf = sb(es, [128, SV], BF16, "onesbf")
                    LP = [sb(es, [128, SV + 1], F32, "LP") for _ in range(NLP)]
                else:
                    b0m = sb(es, [128, TABW], F32, "b0m")
                    mb = sb(es, [128, QT, NBLK], F32, "mb")
                    ind = sb(es, [NBLK, SV], BF16, "ind")
                    q32 = [sb(es, [128, 128], F32, "q32") for _ in range(NBUF)]
                    kms2 = [sb(es, [128, NBLK], F32, "kms") for _ in range(2)]
                    selb = [sb(es, [128, NBLK], BF16, "selb") for _ in range(NBUF)]
                    selbT = [sb(es, [NBLK, 128], BF16, "selbT") for _ in range(NBUF)]
                    osb = [sb(es, [128, 128], BF16, "osb") for _ in range(NBUF)]
                zb = [ps(es, [128, 512], F32, "z") for _ in range(3)]
                trbF = [ps(es, [128, 8, 128], BF16, "tr") for _ in range(2)]
                trb = [t[:, 0:4, :] for t in trbF]
                obF = ps(es, [128, 512], F32, "o")
                ob = obF[:, 0:128]
                gbkF = ps(es, [128, 512], F32, "gt")
                gbk = gbkF[:, 0:NBLK]
                mscF = ps(es, [128, 8, 128], BF16, "msc")
                msc = mscF[:, 0, :]

                constb, vb_ = Buf("const"), Buf("v")
                qb2, kb2, Vb2, kmsb2 = ([Buf(n + str(i)) for i in range(2)] for n in ("q", "k", "V", "kms"))
                ahb = [Buf("ah0"), Buf("ah1")]
                Eb, LPb, Wb, WTb, smb, selbb, selbTb, osbb, q32b = (
                    [Buf(n + str(i)) for i in range(8)]
                    for n in ("E", "LP", "W", "WT", "sm", "selb", "selbT", "osb", "q32"))
                zbb = [Buf("z%d" % i) for i in range(3)]
                trbb = [Buf("tr%d" % i) for i in range(2)]
                obb, gbkb, mscb = Buf("o"), Buf("gbk"), Buf("msc")
                st = {"z": 0, "tr": 0, "ev": 0}

                def nxt(kind_, n):
                    i = st[kind_] % n
                    st[kind_] += 1
                    return i

                if first:
                    for p_ in range(cfg.NP):
                        S.cc_op((lambda e, p_=p_: e.collective_compute(
                            "AllGather", ALU.bypass, replica_groups=groups,
                            ins=[kvloc_t.ap()[p_ * 512:(p_ + 1) * 512, :].opt()], outs=[kvall_t[p_].ap().opt()])),
                            writes=[pieceb[p_]])
                S.op("pool", lambda e: e.dma_start(out=ident[:], in_=cid), writes=[constb], dma=True)
                if is_sb:
                    S.op("pool", lambda e: e.dma_start(out=negm[:], in_=cms), writes=[constb], dma=True)
                    S.op("dve", lambda e: e.memset(ones_bf[:], 1.0), writes=[constb])
                    for i in range(NLP):
                        S.op("dve", (lambda e, i=i: e.memset(LP[i][:, 0:1], 0.0)), writes=[LPb[i]])
                else:
                    S.op("pool", lambda e: e.dma_start(out=ind[:], in_=cind), writes=[constb], dma=True)
                    S.op("sp", lambda e: e.dma_start(out=b0m[:], in_=cb0), writes=[constb], dma=True)
                    S.op("sp", lambda e: e.dma_start(out=mb[:], in_=mbias), writes=[constb], dma=True)

                def evac(fn_act, fn_dve, reads, writes):
                    if nxt("ev", 2) == 0:
                        S.op("act", fn_act, reads=reads, writes=writes)
                    else:
                        S.op("dve", fn_dve, reads=reads, writes=writes)

                def load_head(qidx, oi, hb_):
                    pc = oi // 2
                    qT, kT, V = qT2[hb_], kT2[hb_], V2[hb_]
                    qb, kb, Vb = qb2[hb_], kb2[hb_], Vb2[hb_]
                    S.op("sp", lambda e: e.dma_start(out=qT[:], in_=qTs[qidx, :, :]), writes=[qb], dma=True)
                    for kv, dstt, dstb in ((0, kT, kb), (1, vT, vb_)):
                        ro = ((2 * oi + kv) % 4) * 128
                        for r_ in range(2):
                            S.op("sp", (lambda e, dstt=dstt, ro=ro, r_=r_: e.dma_start(
                                out=dstt[:].rearrange("p (i r c) -> p i r c", r=2, c=128)[:, :, r_, :],
                                in_=kvall[pc][r_ * 512 + ro:r_ * 512 + ro + 128, :].rearrange("p (i c) -> p i c", c=128))),
                                 reads=[pieceb[pc]], writes=[dstb], dma=True)
                    for g in range(SV // 512):
                        ti = nxt("tr", 2)

                        def tr(e, g=g, ti=ti):
                            ins = None
                            for jj in range(4):
                                j = 4 * g + jj
                                ins = e.transpose(trb[ti][:, jj, :], vT[:, j * 128:(j + 1) * 128], ident[:])
                            return ins
                        S.op("pe", tr, reads=[vb_, constb], writes=[trbb[ti]])
                        evac((lambda e, g=g, ti=ti: e.activation(out=V[:, 4 * g:4 * g + 4, :], in_=trb[ti], func=AF.Copy)),
                             (lambda e, g=g, ti=ti: e.tensor_copy(out=V[:, 4 * g:4 * g + 4, :], in_=trb[ti])),
                             [trbb[ti]], [Vb])

                def transposes(wi, ti_, n):
                    nt = n // 128
                    for g in range(-(-nt // 4)):
                        ti = nxt("tr", 2)
                        jn = min(4, nt - 4 * g)

                        def tr(e, g=g, ti=ti, jn=jn):
                            ins = None
                            for jj in range(jn):
                                j = 4 * g + jj
                                ins = e.transpose(trb[ti][:, jj, :], W[wi][:, j * 128:(j + 1) * 128], ident[:])
                            return ins
                        S.op("pe", tr, reads=[Wb[wi], constb], writes=[trbb[ti]])
                        evac((lambda e, g=g, ti=ti, jn=jn: e.activation(
                            out=WT[ti_][:, 4 * g:4 * g + jn, :], in_=trb[ti][:, 0:jn, :], func=AF.Copy)),
                             (lambda e, g=g, ti=ti, jn=jn: e.tensor_copy(
                                 out=WT[ti_][:, 4 * g:4 * g + jn, :], in_=trb[ti][:, 0:jn, :])),
                             [trbb[ti]], [WTb[ti_]])

                def pipeline(stage_lists):
                    bystep = {}
                    for i, x in enumerate(stage_lists):
                        for (l, o, f) in x:
                            bystep.setdefault(i + l, []).append((o, i, f))
                    for step in sorted(bystep):
                        for o, i, f in sorted(bystep[step], key=lambda z: (z[0], z[1])):
                            f()

                tcount = 0
                tiles = []
                for hd in range(H):
                    ahi = hd % 2
                    hb_ = hd % 2
                    qT, kT, V = qT2[hb_], kT2[hb_], V2[hb_]
                    qb, kb, Vb = qb2[hb_], kb2[hb_], Vb2[hb_]
                    if is_sb:
                        loadfn = (lambda hd=hd, hb_=hb_: load_head(H + hd, hd, hb_))
                        storefn = (lambda hd=hd, ahi=ahi: S.op(
                            "sp", (lambda e: e.dma_start(out=attnT[H + hd, :, :], in_=ah[ahi][:])),
                            reads=[ahb[ahi]], dma=True))
                        for i in range(QT):
                            tc = tcount
                            tcount += 1
                            ei, li, wi, ti_, si = tc % NE, tc % NLP, tc % NW, tc % NWT, tc % NSM
                            n = 256 * (i + 1)

                            def s0(i=i, ei=ei, li=li, n=n, qT=qT, kT=kT, qb=qb, kb=kb):
                                nch = -(-n // 512)
                                for c in range(nch):
                                    w = min(512, n - 512 * c)
                                    zi = nxt("z", 3)
                                    last = c == nch - 1

                                    def zmm(e, zi=zi, c=c, w=w, last=last):
                                        ins = e.matmul(zb[zi][:, 0:w], lhsT=qT[:, i * 128:(i + 1) * 128],
                                                       rhs=kT[:, 512 * c:512 * c + w], start=True, stop=not last)
                                        if last:
                                            ins = e.matmul(zb[zi][:, w - 256:w], lhsT=ident[:], rhs=negm[:],
                                                           start=False, stop=True)
                                        return ins
                                    S.op("pe", zmm, reads=[qb, kb, constb], writes=[zbb[zi]])
                                    S.op("act", (lambda e, zi=zi, c=c, w=w: e.activation(
                                        out=E[ei][:, 512 * c:512 * c + w], in_=zb[zi][:, 0:w], func=AF.Exp, scale=scale)),
                                         reads=[zbb[zi]], writes=[Eb[ei]])
                                S.op("act", (lambda e: e.activation(out=LP[li][:, 1:n + 1], in_=E[ei][:, 0:n],
                                                                    func=AF.Ln, bias=1.0, scale=1.0)),
                                     reads=[Eb[ei]], writes=[LPb[li]])

                            def s1(li=li, si=si, n=n):
                                S.op("dve", (lambda e: e.tensor_tensor_scan(
                                    out=LP[li][:, 1:n + 1], data0=ones_bf[:, 0:n], data1=LP[li][:, 1:n + 1], initial=0.0,
                                    op0=ALU.mult, op1=ALU.add)),
                                     reads=[LPb[li], constb], writes=[LPb[li]])
                                S.op("dve", (lambda e: e.tensor_scalar(
                                    out=sm[si][:, 0:1], in0=LP[li][:, n:n + 1], scalar1=-1.0, scalar2=None, op0=ALU.mult)),
                                     reads=[LPb[li]], writes=[smb[si]])

                            def s2(li=li, wi=wi, si=si, n=n):
                                S.op("act", (lambda e: e.activation(out=W[wi][:, 0:n], in_=LP[li][:, 0:n],
                                                                    func=AF.Exp, bias=sm[si][:, 0:1], scale=1.0)),
                                     reads=[LPb[li], smb[si]], writes=[Wb[wi]])

                            def s3(ei=ei, wi=wi, n=n):
                                S.op("dve", (lambda e: e.tensor_tensor(
                                    out=W[wi][:, 0:n], in0=E[ei][:, 0:n], in1=W[wi][:, 0:n], op=ALU.mult)),
                                     reads=[Eb[ei], Wb[wi]], writes=[Wb[wi]])

                            def s4(i=i, wi=wi, ti_=ti_, n=n, ahi=ahi, V=V, Vb=Vb):
                                transposes(wi, ti_, n)
                                nt = n // 128

                                def pv(e):
                                    ins = None
                                    for j in range(nt):
                                        ins = e.matmul(ob, lhsT=V[:, j, :], rhs=WT[ti_][:, j, :],
                                                       start=(j == 0), stop=(j == nt - 1))
                                    return ins
                                S.op("pe", pv, reads=[Vb, WTb[ti_]], writes=[obb])
                                S.op("act", (lambda e: e.activation(out=ah[ahi][:, i * 128:(i + 1) * 128], in_=ob,
                                                                    func=AF.Copy)),
                                     reads=[obb], writes=[ahb[ahi]])
                            stg = [(0, 3, s0), (1, 2, s1), (2, 0, s2), (3, 1, s3), (4, 4, s4)]
                            if i == 0:
                                stg.append((-2, -1, loadfn))
                            if i == QT - 1:
                                stg.append((5, 9, storefn))
                            tiles.append(stg)
                        continue

                    m_h = alibi_slope(hd, H)
                    kms, kmsb = kms2[hb_], kmsb2[hb_]

                    def loadfn(hd=hd, hb_=hb_, kT=kT, kb=kb, kms=kms, kmsb=kmsb):
                        load_head(hd, H + hd, hb_)
                        S.op("dve", lambda e: e.tensor_reduce(out=kms[:], in_=kT[:].rearrange("p (n l) -> p n l", l=256),
                                                              axis=AX.X, op=ALU.add),
                             reads=[kb], writes=[kmsb])
                    storefn = (lambda hd=hd, ahi=ahi: S.op(
                        "sp", (lambda e: e.dma_start(out=attnT[hd, :, :], in_=ah[ahi][:])),
                        reads=[ahb[ahi]], dma=True))
                    for i in range(QT):
                        tc = tcount
                        tcount += 1
                        ei, wi, ti_, si, qi = tc % NE, tc % NW, tc % NWT, tc % NSM, tc % 2
                        vb = i
                        nk = (vb + 1) * 256
                        c0 = C0 - 256 * i

                        def m0a(i=i, si=si, qi=qi, vb=vb, qT=qT, qb=qb, kms=kms, kmsb=kmsb):
                            S.op("pool", (lambda e: e.tensor_copy(out=q32[qi][:], in_=qT[:, i * 128:(i + 1) * 128])),
                                 reads=[qb], writes=[q32b[qi]])
                            S.op("pe", (lambda e: e.matmul(gbk, lhsT=q32[qi][:], rhs=kms[:], start=True, stop=True)),
                                 reads=[q32b[qi], kmsb], writes=[gbkb])
                            S.op("dve", (lambda e: e.tensor_tensor(out=sm[si][:, 0:NBLK], in0=gbk,
                                                                   in1=mb[:, i, :], op=ALU.add)),
                                 reads=[gbkb, constb], writes=[smb[si]])
                            if NBLK < 8:
                                S.op("dve", (lambda e: e.memset(sm[si][:, NBLK:8], -3.0e38)),
                                     reads=[smb[si]], writes=[smb[si]])
                            S.op("dve", (lambda e: e.max(out=sm[si][:, 16:24], in_=sm[si][:, 0:max(NBLK, 8)])),
                                 reads=[smb[si]], writes=[smb[si]])
                            S.op("dve", (lambda e: e.tensor_scalar(
                                out=sm[si][:, 24:24 + NBLK], in0=sm[si][:, 0:NBLK], scalar1=sm[si][:, 18:19],
                                scalar2=BIG / scale, op0=ALU.is_ge, op1=ALU.mult)),
                                 reads=[smb[si]], writes=[smb[si]])
                            S.op("dve", (lambda e: e.scalar_tensor_tensor(
                                out=selb[qi][:], in0=sm[si][:, 24:24 + NBLK], scalar=-BIG / scale, in1=mb[:, i, :],
                                op0=ALU.add, op1=ALU.add)),
                                 reads=[smb[si], constb], writes=[selbb[qi]])
                            S.op("dve", (lambda e: e.memset(selb[qi][:, vb:vb + 1], 0.0)),
                                 reads=[selbb[qi]], writes=[selbb[qi]])

                        def m0b(qi=qi):
                            S.op("pe", (lambda e: e.transpose(msc[0:NBLK, :], selb[qi][:], ident[:])),
                                 reads=[selbb[qi], constb], writes=[mscb])
                            S.op("act", (lambda e: e.activation(out=selbT[qi][:], in_=msc[0:NBLK, :], func=AF.Copy)),
                                 reads=[mscb], writes=[selbTb[qi]])

                        def m1(i=i, ei=ei, qi=qi, nk=nk, c0=c0, m_h=m_h, qT=qT, kT=kT, qb=qb, kb=kb):
                            for c in range(-(-nk // 512)):
                                w = min(512, nk - 512 * c)
                                zi = nxt("z", 3)

                                def zmm(e, zi=zi, c=c, w=w):
                                    e.matmul(zb[zi][:, 0:w], lhsT=qT[:, i * 128:(i + 1) * 128],
                                             rhs=kT[:, 512 * c:512 * c + w], start=True, stop=False)
                                    return e.matmul(zb[zi][:, 0:w], lhsT=selbT[qi][:],
                                                    rhs=ind[:, 512 * c:512 * c + w], start=False, stop=True)
                                S.op("pe", zmm, reads=[qb, kb, selbTb[qi], constb], writes=[zbb[zi]])
                                S.op("dve", (lambda e, zi=zi, c=c, w=w: e.scalar_tensor_tensor(
                                    out=E[ei][:, 512 * c:512 * c + w], in0=b0m[:, c0 + 512 * c:c0 + 512 * c + w],
                                    scalar=-m_h / scale, in1=zb[zi][:, 0:w], op0=ALU.mult, op1=ALU.add)),
                                     reads=[zbb[zi], constb], writes=[Eb[ei]])

                        def m2a(ei=ei, si=si, nk=nk):
                            S.op("dve", (lambda e: e.tensor_reduce(out=sm[si][:, 56:57], in_=E[ei][:, 0:nk],
                                                                   axis=AX.X, op=ALU.max)),
                                 reads=[Eb[ei]], writes=[smb[si]])
                            S.op("dve", (lambda e: e.tensor_scalar(out=sm[si][:, 57:58], in0=sm[si][:, 56:57],
                                                                   scalar1=-scale, scalar2=None, op0=ALU.mult)),
                                 reads=[smb[si]], writes=[smb[si]])

                        def m2b(ei=ei, wi=wi, si=si, nk=nk):
                            S.op("act", (lambda e: e.activation(
                                out=W[wi][:, 0:nk], in_=E[ei][:, 0:nk], func=AF.Exp, bias=sm[si][:, 57:58], scale=scale,
                                accum_out=sm[si][:, 58:59])),
                                 reads=[Eb[ei], smb[si]], writes=[Wb[wi], smb[si]])

                        def m3(i=i, wi=wi, ti_=ti_, si=si, qi=qi, nk=nk, ahi=ahi, V=V, Vb=Vb):
                            S.op("dve", (lambda e: e.reciprocal(out=sm[si][:, 59:60], in_=sm[si][:, 58:59])),
                                 reads=[smb[si]], writes=[smb[si]])
                            transposes(wi, ti_, nk)
                            nt = nk // 128

                            def pv(e):
                                ins = None
                                for j in range(nt):
                                    ins = e.matmul(ob, lhsT=WT[ti_][:, j, :], rhs=V[:, j, :],
                                                   start=(j == 0), stop=(j == nt - 1))
                                return ins
                            S.op("pe", pv, reads=[Vb, WTb[ti_]], writes=[obb])
                            S.op("dve", (lambda e: e.tensor_scalar(out=osb[qi][:], in0=ob, scalar1=sm[si][:, 59:60],
                                                                   scalar2=None, op0=ALU.mult)),
                                 reads=[obb, smb[si]], writes=[osbb[qi]])
                            S.op("pe", (lambda e: e.transpose(msc, osb[qi][:], ident[:])),
                                 reads=[osbb[qi], constb], writes=[mscb])
                            S.op("dve", (lambda e: e.tensor_copy(out=ah[ahi][:, i * 128:(i + 1) * 128], in_=msc)),
                                 reads=[mscb], writes=[ahb[ahi]])
                        stg = [(0, 0, m0a), (1, 1, m1), (2, 2, m2a), (3, 3, m2b), (4, 4, m3), (0, 5, m0b)]
                        if i == 0:
                            stg.append((-2, -1, loadfn))
                        if i == QT - 1:
                            stg.append((5, 9, storefn))
                        tiles.append(stg)
                pipeline(tiles)
                S.drain_dmas("sp")
                with nc.Block() as block:
                    S.emit(block)

        pieceb = [Buf("piece%d" % p_) for p_ in range(cfg.NP)]
        phase_ac("A")
        phase_b("sb", True)
        phase_b("moba", False)
        phase_ac("C")
    return nc


def tile_w(W):
    K, N = W.shape
    return np.ascontiguousarray(W.reshape(K // 128, 128, N // 128, 128).transpose(2, 1, 0, 3))


def fm(a):
    T, C = a.shape
    return np.ascontiguousarray(a.T.reshape(C // 128, 128, T).transpose(1, 0, 2))


def consts(cfg, hf):
    t = np.arange(128)[:, None]
    ident = (t == np.arange(128)[None, :]).astype(np.float32)
    strict = (np.arange(128)[None, :] < t).astype(np.float32)
    if hf == 0:
        mask2 = np.concatenate([strict, np.zeros((128, 128), np.float32)], axis=1)
    else:
        mask2 = np.concatenate([np.ones((128, 128), np.float32), strict], axis=1)
    mask2 = (mask2 - 1.0) * np.float32(BIG)
    c = np.arange(cfg.TABW)[None, :]
    d = (t + 128 * hf - (c - cfg.C0)).astype(np.float32)
    b0m = np.where(d >= 0, d, np.float32(1.0e9)).astype(np.float32)
    return ident, np.ascontiguousarray(mask2), b0m


def mask_bias(cfg):
    mbv = np.zeros((cfg.QT, cfg.NBLK), np.float32)
    for i in range(cfg.QT):
        mbv[i, i:] = -1.0e30
    return np.ascontiguousarray(np.broadcast_to(mbv[None], (128, cfg.QT, cfg.NBLK)))


def run(cfg, inp):
    D, SH = cfg.D, cfg.SH
    nc = build_program(cfg)
    f32 = np.float32
    wmap = {"f1g": "ffn1_w_gate", "f1u": "ffn1_w_up", "f1d": "ffn1_w_down", "win": "w_in", "wbm": "w_branch_moba",
            "wbs": "w_branch_sb", "wout": "w_out", "f2g": "ffn2_w_gate", "f2u": "ffn2_w_up", "f2d": "ffn2_w_down",
            "wpg": "w_ple_gate", "wpp": "w_ple_proj"}
    shared = {k: tile_w(np.asarray(inp[v], f32)[0]) for k, v in wmap.items()}
    g = np.stack([np.asarray(inp[n], f32).reshape(-1) for n in
                  ("ffn1_norm", "mix_norm", "ffn2_norm", "ple_norm", "final_norm")])
    shared["gall"] = np.ascontiguousarray(g.reshape(5, cfg.KC, 128).transpose(2, 0, 1))
    shared["cind"] = (np.arange(cfg.SV)[None, :] // 256 == np.arange(cfg.NBLK)[:, None]).astype(np.float32)
    shared["mbias"] = mask_bias(cfg)
    x = np.asarray(inp["x"], f32)
    p = np.asarray(inp["p"], f32)[0]
    nb = cfg.QT
    in_maps = []
    for r in range(cfg.NCORES):
        b, hf = r // 2, r % 2
        m = dict(shared)
        m["cident"], m["cmask2"], m["cb0m"] = consts(cfg, hf)
        m["xT"] = fm(x[b].reshape(nb, 2, 128, D)[:, hf].reshape(SH, D))
        m["pT"] = fm(p[b].reshape(nb, 2, 128, -1)[:, hf].reshape(SH, -1))
        in_maps.append(m)
    res = run_bass_kernel_spmd(nc, in_maps, core_ids=list(range(cfg.NCORES)))
    out = np.empty((cfg.B, 2 * SH, D), f32)
    for r in range(cfg.NCORES):
        b, hf = r // 2, r % 2
        oT = np.asarray(res.results[r]["outT"], f32)
        out[b].reshape(nb, 2, 128, D)[:, hf] = oT.transpose(2, 1, 0).reshape(nb, 128, D)
    return out


def kernel(**inputs):
    return run(FULL, inputs)
```

```python
import numpy as np
import ml_dtypes
from contextlib import ExitStack
import concourse.bass as bass
import concourse.mybir as mybir
from concourse.bass_utils import run_bass_kernel_spmd

F32 = mybir.dt.float32
BF16 = mybir.dt.bfloat16
AF = mybir.ActivationFunctionType
ALU = mybir.AluOpType
AX = mybir.AxisListType

RMS_EPS = 1e-6
BIG = 30000.0
NDS = 24
WK = 16


class Cfg:
    def __init__(self, D, F, SH, NT, H, B):
        self.D, self.F, self.SH, self.NT, self.H, self.B = D, F, SH, NT, H, B
        self.KC = D // 128
        self.FC = F // 128
        self.TT = SH // NT
        self.SV = 2 * SH
        self.QT = SH // 128
        self.NBLK = self.SV // 256
        self.NQKV = 6 * H
        self.INC = 6 * H * 128 + 2 * D
        self.C0 = 256 * (self.QT - 1)
        self.TABW = 256 * self.QT
        self.NP = (4 * H) // 4
        self.GC = min(32, self.FC)
        self.NCORES = 2 * B


FULL = Cfg(D=4096, F=11008, SH=2048, NT=512, H=16, B=4)


class Buf:
    __slots__ = ("name", "w", "r")

    def __init__(self, name):
        self.name = name
        self.w = None
        self.r = {}


class Sched:
    def __init__(self, nc, es):
        self.nc = nc
        self.eng = {"pe": nc.tensor, "act": nc.scalar, "dve": nc.vector, "pool": nc.gpsimd, "sp": nc.sync}
        self.sem = {k: es.enter_context(nc.semaphore("s_" + k)) for k in self.eng}
        self.cnt = {k: 0 for k in self.eng}
        self.dsem = [es.enter_context(nc.semaphore("d%d" % i)) for i in range(NDS)]
        self.dcnt = [0] * NDS
        self.dnext = {"sp": 0, "pool": 0, "act": 0}
        self.dbase = {"sp": 0, "pool": NDS // 2, "act": 0}
        self.ops = {k: [] for k in self.eng}
        self.waited = {k: {} for k in self.eng}
        self.csem = []
        self.es = es

    def op(self, e, fn, reads=(), writes=(), dma=False):
        deps = []
        for b in reads:
            if b.w is not None:
                deps.append(b.w)
        for b in writes:
            if b.w is not None:
                deps.append(b.w)
            for k, v in b.r.items():
                deps.append((k, v))
        if dma:
            i = self.dbase[e] + self.dnext[e]
            self.dnext[e] = (self.dnext[e] + 1) % (NDS // 2)
            if self.dcnt[i] > 0:
                deps.append((("d", i), self.dcnt[i]))
            self.dcnt[i] += 16
            ev = (("d", i), self.dcnt[i])
        else:
            self.cnt[e] += 1
            ev = (("e", e), self.cnt[e])
        waits = {}
        wd = self.waited[e]
        for key, val in deps:
            if key == ("e", "pe") and e == "pe" and not dma:
                continue
            if wd.get(key, 0) >= val:
                continue
            if waits.get(key, 0) < val:
                waits[key] = val
        for k, v in waits.items():
            wd[k] = v
        self.ops[e].append((list(waits.items()), fn, ev))
        for b in reads:
            if b.r.get(ev[0], 0) < ev[1]:
                b.r[ev[0]] = ev[1]
        for b in writes:
            b.w = ev
            b.r = {}
        return ev

    def cc_op(self, fn, writes):
        i = len(self.csem)
        self.csem.append(self.es.enter_context(self.nc.semaphore("cc%d" % i)))
        ev = (("c", i), 1)
        self.ops["pool"].append(([], fn, ev))
        for b in writes:
            b.w = ev
            b.r = {}
        return ev

    def drain_dmas(self, e="sp"):
        waits = []
        for i in range(NDS):
            if self.dcnt[i] > self.waited[e].get(("d", i), 0):
                waits.append((("d", i), self.dcnt[i]))
                self.waited[e][("d", i)] = self.dcnt[i]
        self.ops[e].append((waits, None, None))

    def _semof(self, key):
        if key[0] == "c":
            return self.csem[key[1]]
        return self.dsem[key[1]] if key[0] == "d" else self.sem[key[1]]

    def emit(self, block):
        def run(name):
            def body(engine):
                for waits, fn, ev in self.ops[name]:
                    for key, val in waits:
                        engine.wait_ge(self._semof(key), val)
                    if fn is None:
                        continue
                    inst = fn(engine)
                    if ev[0][0] == "c":
                        inst.then_inc(self._semof(ev[0]))
                    else:
                        inst.then_inc(self._semof(ev[0]), 16 if ev[0][0] == "d" else 1)
            return body
        for name, dec in (("sp", block.sync), ("pe", block.tensor), ("act", block.scalar),
                          ("dve", block.vector), ("pool", block.gpsimd)):
            if self.ops[name]:
                dec(run(name))
            self.ops[name] = []


def alibi_slope(h, H):
    return float(2.0 ** (-8.0 * (h + 1) / H))


def build_program(cfg):
    D, F, SH, NT, H = cfg.D, cfg.F, cfg.SH, cfg.NT, cfg.H
    KC, FC, TT, SV, QT, NBLK = cfg.KC, cfg.FC, cfg.TT, cfg.SV, cfg.QT, cfg.NBLK
    C0, TABW, GC = cfg.C0, cfg.TABW, cfg.GC
    scale = 128.0 ** -0.5

    nc = bass.Bass("TRN2", target_bir_lowering=False)

    def din(name, shape, dt=F32):
        return nc.dram_tensor(name, list(shape), dt, kind="ExternalInput").ap()

    xT = din("xT", [128, KC, SH])
    pT = din("pT", [128, 2, SH])
    gall = din("gall", [128, 5, KC])
    mbias = din("mbias", [128, QT, NBLK])
    cid = din("cident", [128, 128])
    cms = din("cmask2", [128, 256])
    cb0 = din("cb0m", [128, TABW])
    cind = din("cind", [NBLK, SV])
    Wd_ = {}
    for nm, K, N in (("f1g", D, F), ("f1u", D, F), ("f1d", F, D), ("win", D, cfg.INC), ("wbm", H * 128, D),
                     ("wbs", H * 128, D), ("wout", D, D), ("f2g", D, F), ("f2u", D, F), ("f2d", F, D),
                     ("wpg", D, D), ("wpp", 256, D)):
        Wd_[nm] = din(nm, [N // 128, 128, K // 128, 128])
    outT = nc.dram_tensor("outT", [128, KC, SH], F32, kind="ExternalOutput").ap()
    h1 = nc.dram_tensor("h1s", [128, KC, SH], F32, kind="Internal").ap()
    qTs = nc.dram_tensor("qTs", [2 * H, 128, SH], BF16, kind="Internal").ap()
    kvloc_t = nc.dram_tensor("kvloc", [4 * H * 128, SH], BF16)
    kvloc = kvloc_t.ap()
    kvall_t = [nc.dram_tensor("kvall%d" % p_, [2 * 512, SH], BF16) for p_ in range(cfg.NP)]
    kvall = [t_.ap() for t_ in kvall_t]
    groups = [[2 * b_, 2 * b_ + 1] for b_ in range(cfg.B)]

    def kv_slot(j):
        if 4 * H <= j < 5 * H:
            return 2 * (j - 4 * H)
        if 5 * H <= j < 6 * H:
            return 2 * (j - 5 * H) + 1
        if H <= j < 2 * H:
            return 2 * (H + j - H)
        return 2 * (H + j - 2 * H) + 1
    attnT = nc.dram_tensor("attns", [2 * H, 128, SH], BF16, kind="Internal").ap()

    with ExitStack() as es0:
        S = Sched(nc, es0)
        cnt = [0]

        def sb(es, shape, dt, nm=None):
            cnt[0] += 1
            return es.enter_context(nc.sbuf_tensor("%s_%d" % (nm or "t", cnt[0]), list(shape), dt))

        def ps(es, shape, dt, nm=None):
            cnt[0] += 1
            return es.enter_context(nc.psum_tensor("%s_%d" % (nm or "p", cnt[0]), list(shape), dt))

        def phase_ac(which):
            with ExitStack() as es:
                h = sb(es, [128, KC, NT], F32, "h")
                xn = sb(es, [128, KC, NT], BF16, "xn")
                big2 = sb(es, [128, max(GC, 2 * H), NT], BF16, "big2")
                gated = sb(es, [128, KC, NT], BF16, "gated") if which == "C" else None
                NWS = 6
                wring = [sb(es, [128, WK, 128], BF16, "w") for _ in range(NWS)]
                gsb = sb(es, [128, 5, KC], F32, "g")
                ones32 = sb(es, [128, 128], F32, "ones")
                sq = [sb(es, [128, NT], F32, "sq") for _ in range(2)]
                rs = sb(es, [128, NT], F32, "rs")
                rs2 = sb(es, [128, NT], F32, "rs2")
                tmp = [sb(es, [128, NT], F32, "tmp") for _ in range(4)]
                stage = [sb(es, [128, NT], BF16, "st") for _ in range(4)]
                pbf = sb(es, [128, 2, NT], BF16, "pbf")
                banksF = [ps(es, [128, 512], F32, "bk") for _ in range(8)]
                banks = [b[:, 0:NT] for b in banksF]

                hb = [Buf("h%d" % c) for c in range(KC)]
                xnb = [Buf("xn%d" % c) for c in range(KC)]
                b2b = [Buf("b2_%d" % c) for c in range(max(GC, 2 * H))]
                gtb = [Buf("gt%d" % c) for c in range(KC)]
                wrb = [Buf("w%d" % i) for i in range(NWS)]
                sqb = [Buf("sq0"), Buf("sq1")]
                rsb, rs2b, gb, onesb, pbfb = Buf("rs"), Buf("rs2"), Buf("g"), Buf("ones"), Buf("pbf")
                tmpb = [Buf("tmp%d" % i) for i in range(4)]
                stb = [Buf("st%d" % i) for i in range(4)]
                bkb = [Buf("bk%d" % i) for i in range(8)]
                st = {"w": 0, "tmp": 0, "st": 0, "bk": {}}

                S.op("sp", lambda e: e.dma_start(out=gsb[:], in_=gall), writes=[gb], dma=True)
                S.op("dve", lambda e: e.memset(ones32[:], 1.0), writes=[onesb])

                def mm_group(bank_i, Wt, j, k0, k1, rhs_fn, rhs_bufs):
                    k = k0
                    while k < k1:
                        kk = min(k + WK, k1)
                        si = st["w"] % NWS
                        st["w"] += 1
                        wt, wb = wring[si], wrb[si]
                        S.op("pool", (lambda e, wt=wt, k=k, kk=kk: e.dma_start(
                            out=wt[:, 0:kk - k, :], in_=Wt[j, :, k:kk, :])), writes=[wb], dma=True)

                        def mm(e, wt=wt, k=k, kk=kk):
                            ins = None
                            for q in range(k, kk):
                                ins = e.matmul(banks[bank_i], lhsT=wt[:, q - k, :], rhs=rhs_fn(q),
                                               start=(q == k0), stop=(q == k1 - 1))
                            return ins
                        S.op("pe", mm, reads=[wb] + [rhs_bufs[q] for q in range(k, kk)], writes=[bkb[bank_i]])
                        k = kk

                def nxt(kind, n):
                    i = st[kind] % n
                    st[kind] += 1
                    return i

                def norm(gidx, out_t, out_b):
                    NB = 6
                    for c in range(KC):
                        s = c % 2
                        S.op("act", (lambda e, c=c, s=s: e.activation(out=sq[s][:], in_=h[:, c, :], func=AF.Square)),
                             reads=[hb[c]], writes=[sqb[s]])
                        S.op("pe", (lambda e, c=c, s=s: e.matmul(banks[NB], lhsT=ones32[:], rhs=sq[s][:],
                                                                start=(c == 0), stop=(c == KC - 1))),
                             reads=[sqb[s], onesb], writes=[bkb[NB]])
                    S.op("act", lambda e: e.activation(out=rs[:], in_=banks[NB], func=AF.Sqrt,
                                                       bias=RMS_EPS, scale=1.0 / D),
                         reads=[bkb[NB]], writes=[rsb])
                    S.op("dve", lambda e: e.reciprocal(out=rs2[:], in_=rs[:]), reads=[rsb], writes=[rs2b])
                    for c in range(KC):
                        S.op("dve", (lambda e, c=c: e.scalar_tensor_tensor(
                            out=out_t[:, c, :], in0=h[:, c, :], scalar=gsb[:, gidx, c:c + 1], in1=rs2[:],
                            op0=ALU.mult, op1=ALU.mult)),
                             reads=[hb[c], gb, rs2b], writes=[out_b[c]])

                def ffn(wg, wu, wdn, gidx):
                    norm(gidx, xn, xnb)
                    f0 = 0
                    ng = -(-FC // GC)
                    gs = -(-FC // ng)
                    while f0 < FC:
                        f1 = min(FC, f0 + gs)
                        for f in range(f0, f1):
                            bg = (f % 2)
                            bu = 2 + (f % 2)
                            mm_group(bg, wg, f, 0, KC, lambda q: xn[:, q, :], xnb)
                            mm_group(bu, wu, f, 0, KC, lambda q: xn[:, q, :], xnb)
                            ti = nxt("tmp", 4)
                            S.op("act", (lambda e, bg=bg, ti=ti: e.activation(out=tmp[ti][:], in_=banks[bg],
                                                                            func=AF.Silu)),
                                 reads=[bkb[bg]], writes=[tmpb[ti]])
                            S.op("dve", (lambda e, bu=bu, ti=ti, f=f, f0=f0: e.tensor_tensor(
                                out=big2[:, f - f0, :], in0=banks[bu], in1=tmp[ti][:], op=ALU.mult)),
                                 reads=[bkb[bu], tmpb[ti]], writes=[b2b[f - f0]])
                        for c in range(KC):
                            bd = 4 + (c % 2)
                            mm_group(bd, wdn, c, f0, f1, (lambda q, f0=f0: big2[:, q - f0, :]),
                                     {q: b2b[q - f0] for q in range(f0, f1)})
                            S.op("dve", (lambda e, bd=bd, c=c: e.scalar_tensor_tensor(
                                out=h[:, c, :], in0=banks[bd], scalar=0.5, in1=h[:, c, :],
                                op0=ALU.mult, op1=ALU.add)),
                                 reads=[bkb[bd], hb[c]], writes=[hb[c]])
                        f0 = f1

                if which == "A":
                    for vt in range(TT):
                        own = True
                        t0 = vt * NT
                        S.op("sp", (lambda e, t0=t0: e.dma_start(out=h[:], in_=xT[:, :, t0:t0 + NT])),
                             writes=hb, dma=True)
                        ffn(Wd_["f1g"], Wd_["f1u"], Wd_["f1d"], 0)
                        if own:
                            S.op("sp", (lambda e, t0=t0: e.dma_start(out=h1[:, :, t0:t0 + NT], in_=h[:])),
                                 reads=hb, dma=True)
                        norm(1, xn, xnb)
                        chunks = list(range(6 * H)) if own else (list(range(H, 3 * H)) + list(range(4 * H, 6 * H)))
                        for n_, j in enumerate(chunks):
                            bk = n_ % 4
                            mm_group(bk, Wd_["win"], j, 0, KC, lambda q: xn[:, q, :], xnb)
                            si = nxt("st", 4)
                            if n_ % 2 == 0:
                                S.op("act", (lambda e, bk=bk, si=si: e.activation(out=stage[si][:], in_=banks[bk],
                                                                                func=AF.Copy)),
                                     reads=[bkb[bk]], writes=[stb[si]])
                            else:
                                S.op("dve", (lambda e, bk=bk, si=si: e.tensor_copy(out=stage[si][:], in_=banks[bk])),
                                     reads=[bkb[bk]], writes=[stb[si]])
                            if j < H or 3 * H <= j < 4 * H:
                                dst = qTs[j if j < H else j - 2 * H, :, t0:t0 + NT]
                            else:
                                sl = kv_slot(j)
                                dst = kvloc[sl * 128:(sl + 1) * 128, t0:t0 + NT]
                            S.op("sp", (lambda e, dst=dst, si=si: e.dma_start(out=dst, in_=stage[si][:])),
                                 reads=[stb[si]], dma=True)
                else:
                    for t in range(TT):
                        t0 = t * NT
                        S.op("sp", (lambda e, t0=t0: e.dma_start(out=h[:], in_=h1[:, :, t0:t0 + NT])),
                             writes=hb, dma=True)
                        S.op("sp", (lambda e, t0=t0: e.dma_start(
                            out=big2[:, 0:2 * H, :], in_=attnT[:, :, t0:t0 + NT].rearrange("j p t -> p j t"))),
                             writes=b2b[0:2 * H], dma=True)
                        S.op("pool", (lambda e, t0=t0: e.dma_start(out=pbf[:], in_=pT[:, :, t0:t0 + NT])),
                             writes=[pbfb], dma=True)
                        norm(1, xn, xnb)
                        for j in range(KC):
                            mm_group(0, Wd_["win"], 6 * H + j, 0, KC, lambda q: xn[:, q, :], xnb)
                            mm_group(1, Wd_["win"], 6 * H + KC + j, 0, KC, lambda q: xn[:, q, :], xnb)
                            mm_group(2, Wd_["wbm"], j, 0, H, lambda q: big2[:, q, :], b2b)
                            mm_group(3, Wd_["wbs"], j, 0, H, lambda q: big2[:, H + q, :],
                                     {q: b2b[H + q] for q in range(H)})
                            ta, tb = nxt("tmp", 4), nxt("tmp", 4)
                            S.op("act", (lambda e, ta=ta: e.activation(out=tmp[ta][:], in_=banks[0], func=AF.Sigmoid)),
                                 reads=[bkb[0]], writes=[tmpb[ta]])
                            S.op("act", (lambda e, tb=tb: e.activation(out=tmp[tb][:], in_=banks[1], func=AF.Sigmoid)),
                                 reads=[bkb[1]], writes=[tmpb[tb]])
                            S.op("dve", (lambda e, ta=ta: e.tensor_tensor(out=tmp[ta][:], in0=banks[2], in1=tmp[ta][:],
                                                                         op=ALU.mult)),
                                 reads=[bkb[2], tmpb[ta]], writes=[tmpb[ta]])
                            S.op("dve", (lambda e, tb=tb: e.tensor_tensor(out=tmp[tb][:], in0=banks[3], in1=tmp[tb][:],
                                                                         op=ALU.mult)),
                                 reads=[bkb[3], tmpb[tb]], writes=[tmpb[tb]])
                            S.op("dve", (lambda e, ta=ta, tb=tb, j=j: e.tensor_tensor(
                                out=gated[:, j, :], in0=tmp[ta][:], in1=tmp[tb][:], op=ALU.add)),
                                 reads=[tmpb[ta], tmpb[tb]], writes=[gtb[j]])
                        for c in range(KC):
                            bd = 4 + (c % 2)
                            mm_group(bd, Wd_["wout"], c, 0, KC, lambda q: gated[:, q, :], gtb)
                            S.op("dve", (lambda e, bd=bd, c=c: e.tensor_tensor(out=h[:, c, :], in0=banks[bd],
                                                                             in1=h[:, c, :], op=ALU.add)),
                                 reads=[bkb[bd], hb[c]], writes=[hb[c]])
                        ffn(Wd_["f2g"], Wd_["f2u"], Wd_["f2d"], 2)
                        norm(3, xn, xnb)
                        for c in range(KC):
                            bg, bu = (c % 2), 2 + (c % 2)
                            mm_group(bg, Wd_["wpg"], c, 0, KC, lambda q: xn[:, q, :], xnb)
                            mm_group(bu, Wd_["wpp"], c, 0, 2, lambda q: pbf[:, q, :], {0: pbfb, 1: pbfb})
                            ti = nxt("tmp", 4)
                            S.op("act", (lambda e, bg=bg, ti=ti: e.activation(out=tmp[ti][:], in_=banks[bg],
                                                                            func=AF.Sigmoid)),
                                 reads=[bkb[bg]], writes=[tmpb[ti]])
                            S.op("dve", (lambda e, bu=bu, ti=ti: e.tensor_tensor(out=tmp[ti][:], in0=banks[bu],
                                                                               in1=tmp[ti][:], op=ALU.mult)),
                                 reads=[bkb[bu], tmpb[ti]], writes=[tmpb[ti]])
                            S.op("dve", (lambda e, ti=ti, c=c: e.tensor_tensor(out=h[:, c, :], in0=h[:, c, :],
                                                                              in1=tmp[ti][:], op=ALU.add)),
                                 reads=[tmpb[ti], hb[c]], writes=[hb[c]])
                        norm(4, h, hb)
                        S.op("sp", (lambda e, t0=t0: e.dma_start(out=outT[:, :, t0:t0 + NT], in_=h[:])),
                             reads=hb, dma=True)
                S.drain_dmas("sp")
                with nc.Block() as block:
                    S.emit(block)

        NBUF = 3

        def phase_b(kind, first):
            with ExitStack() as es:
                is_sb = kind == "sb"
                ident = sb(es, [128, 128], BF16, "ident")
                qT2 = [sb(es, [128, SH], BF16, "qT") for _ in range(2)]
                kT2 = [sb(es, [128, SV], BF16, "kT") for _ in range(2)]
                vT = sb(es, [128, SV], BF16, "vT")
                V2 = [sb(es, [128, SV // 128, 128], BF16, "V") for _ in range(2)]
                ah = [sb(es, [128, SH], BF16, "ah") for _ in range(2)]
                NE = 3
                NW, NWT, NSM, NLP = 3, 2, 8, 3
                E = [sb(es, [128, SV], F32, "E") for _ in range(NE)]
                W = [sb(es, [128, SV], BF16, "W") for _ in range(NW)]
                WT = [sb(es, [128, SV // 128, 128], BF16, "WT") for _ in range(NWT)]
                sm = [sb(es, [128, 64], F32, "sm") for _ in range(NSM)]
                if is_sb:
                    negm = sb(es, [128, 256], BF16, "negm")
                    ones_bf = sb(es, [128, SV], BF16, "onesbf")
                    LP = [sb(es, [128, SV + 1], F32, "LP") for _ in range(NLP)]
                else:
                    b0m = sb(es, [128, TABW], F32, "b0m")
                    mb = sb(es, [128, QT, NBLK], F32, "mb")
                    ind = sb(es, [NBLK, SV], BF16, "ind")
                    q32 = [sb(es, [128, 128], F32, "q32") for _ in range(NBUF)]
                    kms2 = [sb(es, [128, NBLK], F32, "kms") for _ in range(2)]
                    selb = [sb(es, [128, NBLK], BF16, "selb") for _ in range(NBUF)]
                    selbT = [sb(es, [NBLK, 128], BF16, "selbT") for _ in range(NBUF)]
                    osb = [sb(es, [128, 128], BF16, "osb") for _ in range(NBUF)]
                NZ = 2
                zb = [ps(es, [128, 512], F32, "z") for _ in range(NZ)]
                msc2F = ps(es, [128, 8, 128], BF16, "msc2")
                msc2 = msc2F[:, 0, :]
                trbF = [ps(es, [128, 8, 128], BF16, "tr") for _ in range(2)]
                trb = [t[:, 0:4, :] for t in trbF]
                obF = ps(es, [128, 512], F32, "o")
                ob = obF[:, 0:128]
                gbkF = ps(es, [128, 512], F32, "gt")
                gbk = gbkF[:, 0:NBLK]
                mscF = ps(es, [128, 8, 128], BF16, "msc")
                msc = mscF[:, 0, :]

                constb, vb_ = Buf("const"), Buf("v")
                qb2, kb2, Vb2, kmsb2 = ([Buf(n + str(i)) for i in range(2)] for n in ("q", "k", "V", "kms"))
                ahb = [Buf("ah0"), Buf("ah1")]
                Eb, LPb, Wb, WTb, smb, selbb, selbTb, osbb, q32b = (
                    [Buf(n + str(i)) for i in range(8)]
                    for n in ("E", "LP", "W", "WT", "sm", "selb", "selbT", "osb", "q32"))
                zbb = [Buf("z%d" % i) for i in range(NZ)]
                msc2b = Buf("msc2")
                trbb = [Buf("tr%d" % i) for i in range(2)]
                obb, gbkb, mscb = Buf("o"), Buf("gbk"), Buf("msc")
                st = {"z": 0, "tr": 0, "ev": 0}

                def nxt(kind_, n):
                    i = st[kind_] % n
                    st[kind_] += 1
                    return i

                if first:
                    for p_ in range(cfg.NP):
                        S.cc_op((lambda e, p_=p_: e.collective_compute(
                            "AllGather", ALU.bypass, replica_groups=groups,
                            ins=[kvloc_t.ap()[p_ * 512:(p_ + 1) * 512, :].opt()], outs=[kvall_t[p_].ap().opt()])),
                            writes=[pieceb[p_]])
                S.op("pool", lambda e: e.dma_start(out=ident[:], in_=cid), writes=[constb], dma=True)
                if is_sb:
                    S.op("pool", lambda e: e.dma_start(out=negm[:], in_=cms), writes=[constb], dma=True)
                    S.op("dve", lambda e: e.memset(ones_bf[:], 1.0), writes=[constb])
                    for i in range(NLP):
                        S.op("dve", (lambda e, i=i: e.memset(LP[i][:, 0:1], 0.0)), writes=[LPb[i]])
                else:
                    S.op("pool", lambda e: e.dma_start(out=ind[:], in_=cind), writes=[constb], dma=True)
                    S.op("sp", lambda e: e.dma_start(out=b0m[:], in_=cb0), writes=[constb], dma=True)
                    S.op("sp", lambda e: e.dma_start(out=mb[:], in_=mbias), writes=[constb], dma=True)

                def evac(fn_act, fn_dve, reads, writes):
                    if (not is_sb) or nxt("ev", 2) == 0:
                        S.op("act", fn_act, reads=reads, writes=writes)
                    else:
                        S.op("dve", fn_dve, reads=reads, writes=writes)

                def load_head(qidx, oi, hb_):
                    pc = oi // 2
                    qT, kT, V = qT2[hb_], kT2[hb_], V2[hb_]
                    qb, kb, Vb = qb2[hb_], kb2[hb_], Vb2[hb_]
                    S.op("sp", lambda e: e.dma_start(out=qT[:], in_=qTs[qidx, :, :]), writes=[qb], dma=True)
                    for kv, dstt, dstb in ((0, kT, kb), (1, vT, vb_)):
                        ro = ((2 * oi + kv) % 4) * 128
                        for r_ in range(2):
                            S.op("sp", (lambda e, dstt=dstt, ro=ro, r_=r_: e.dma_start(
                                out=dstt[:].rearrange("p (i r c) -> p i r c", r=2, c=128)[:, :, r_, :],
                                in_=kvall[pc][r_ * 512 + ro:r_ * 512 + ro + 128, :].rearrange("p (i c) -> p i c", c=128))),
                                 reads=[pieceb[pc]], writes=[dstb], dma=True)
                    for g in range(SV // 512):
                        ti = nxt("tr", 2)

                        def tr(e, g=g, ti=ti):
                            ins = None
                            for jj in range(4):
                                j = 4 * g + jj
                                ins = e.transpose(trb[ti][:, jj, :], vT[:, j * 128:(j + 1) * 128], ident[:])
                            return ins
                        S.op("pe", tr, reads=[vb_, constb], writes=[trbb[ti]])
                        evac((lambda e, g=g, ti=ti: e.activation(out=V[:, 4 * g:4 * g + 4, :], in_=trb[ti], func=AF.Copy)),
                             (lambda e, g=g, ti=ti: e.tensor_copy(out=V[:, 4 * g:4 * g + 4, :], in_=trb[ti])),
                             [trbb[ti]], [Vb])

                def transposes(wi, ti_, n):
                    nt = n // 128
                    for g in range(-(-nt // 4)):
                        ti = nxt("tr", 2)
                        jn = min(4, nt - 4 * g)

                        def tr(e, g=g, ti=ti, jn=jn):
                            ins = None
                            for jj in range(jn):
                                j = 4 * g + jj
                                ins = e.transpose(trb[ti][:, jj, :], W[wi][:, j * 128:(j + 1) * 128], ident[:])
                            return ins
                        S.op("pe", tr, reads=[Wb[wi], constb], writes=[trbb[ti]])
                        evac((lambda e, g=g, ti=ti, jn=jn: e.activation(
                            out=WT[ti_][:, 4 * g:4 * g + jn, :], in_=trb[ti][:, 0:jn, :], func=AF.Copy)),
                             (lambda e, g=g, ti=ti, jn=jn: e.tensor_copy(
                                 out=WT[ti_][:, 4 * g:4 * g + jn, :], in_=trb[ti][:, 0:jn, :])),
                             [trbb[ti]], [WTb[ti_]])

                def pipeline(stage_lists):
                    bystep = {}
                    for i, x in enumerate(stage_lists):
                        for (l, o, f) in x:
                            bystep.setdefault(i + l, []).append((o, i, f))
                    for step in sorted(bystep):
                        for o, i, f in sorted(bystep[step], key=lambda z: (z[0], z[1])):
                            f()

                tcount = 0
                tiles = []
                for hd in range(H):
                    ahi = hd % 2
                    hb_ = hd % 2
                    qT, kT, V = qT2[hb_], kT2[hb_], V2[hb_]
                    qb, kb, Vb = qb2[hb_], kb2[hb_], Vb2[hb_]
                    if is_sb:
                        loadfn = (lambda hd=hd, hb_=hb_: load_head(H + hd, hd, hb_))
                        storefn = (lambda hd=hd, ahi=ahi: S.op(
                            "sp", (lambda e: e.dma_start(out=attnT[H + hd, :, :], in_=ah[ahi][:])),
                            reads=[ahb[ahi]], dma=True))
                        for i in range(QT):
                            tc = tcount
                            tcount += 1
                            ei, li, wi, ti_, si = tc % NE, tc % NLP, tc % NW, tc % NWT, tc % NSM
                            n = 256 * (i + 1)

                            def s0(i=i, ei=ei, li=li, n=n, qT=qT, kT=kT, qb=qb, kb=kb):
                                nch = -(-n // 512)
                                for c in range(nch):
                                    w = min(512, n - 512 * c)
                                    zi = nxt("z", NZ)
                                    last = c == nch - 1

                                    def zmm(e, zi=zi, c=c, w=w, last=last):
                                        ins = e.matmul(zb[zi][:, 0:w], lhsT=qT[:, i * 128:(i + 1) * 128],
                                                       rhs=kT[:, 512 * c:512 * c + w], start=True, stop=not last)
                                        if last:
                                            ins = e.matmul(zb[zi][:, w - 256:w], lhsT=ident[:], rhs=negm[:],
                                                           start=False, stop=True)
                                        return ins
                                    S.op("pe", zmm, reads=[qb, kb, constb], writes=[zbb[zi]])
                                    S.op("act", (lambda e, zi=zi, c=c, w=w: e.activation(
                                        out=E[ei][:, 512 * c:512 * c + w], in_=zb[zi][:, 0:w], func=AF.Exp, scale=scale)),
                                         reads=[zbb[zi]], writes=[Eb[ei]])
                                S.op("act", (lambda e: e.activation(out=LP[li][:, 1:n + 1], in_=E[ei][:, 0:n],
                                                                    func=AF.Ln, bias=1.0, scale=1.0)),
                                     reads=[Eb[ei]], writes=[LPb[li]])

                            def s1(li=li, si=si, n=n):
                                S.op("dve", (lambda e: e.tensor_tensor_scan(
                                    out=LP[li][:, 1:n + 1], data0=ones_bf[:, 0:n], data1=LP[li][:, 1:n + 1], initial=0.0,
                                    op0=ALU.mult, op1=ALU.add)),
                                     reads=[LPb[li], constb], writes=[LPb[li]])
                                S.op("dve", (lambda e: e.tensor_scalar(
                                    out=sm[si][:, 0:1], in0=LP[li][:, n:n + 1], scalar1=-1.0, scalar2=None, op0=ALU.mult)),
                                     reads=[LPb[li]], writes=[smb[si]])

                            def s2(li=li, wi=wi, si=si, n=n):
                                S.op("act", (lambda e: e.activation(out=W[wi][:, 0:n], in_=LP[li][:, 0:n],
                                                                    func=AF.Exp, bias=sm[si][:, 0:1], scale=1.0)),
                                     reads=[LPb[li], smb[si]], writes=[Wb[wi]])

                            def s3(ei=ei, wi=wi, n=n):
                                S.op("dve", (lambda e: e.tensor_tensor(
                                    out=W[wi][:, 0:n], in0=E[ei][:, 0:n], in1=W[wi][:, 0:n], op=ALU.mult)),
                                     reads=[Eb[ei], Wb[wi]], writes=[Wb[wi]])

                            def s4(i=i, wi=wi, ti_=ti_, n=n, ahi=ahi, V=V, Vb=Vb):
                                transposes(wi, ti_, n)
                                nt = n // 128

                                def pv(e):
                                    ins = None
                                    for j in range(nt):
                                        ins = e.matmul(ob, lhsT=V[:, j, :], rhs=WT[ti_][:, j, :],
                                                       start=(j == 0), stop=(j == nt - 1))
                                    return ins
                                S.op("pe", pv, reads=[Vb, WTb[ti_]], writes=[obb])
                                S.op("act", (lambda e: e.activation(out=ah[ahi][:, i * 128:(i + 1) * 128], in_=ob,
                                                                    func=AF.Copy)),
                                     reads=[obb], writes=[ahb[ahi]])
                            stg = [(0, 3, s0), (1, 2, s1), (2, 0, s2), (3, 1, s3), (4, 4, s4)]
                            if i == 0:
                                stg.append((-2, -1, loadfn))
                            if i == QT - 1:
                                stg.append((5, 9, storefn))
                            tiles.append(stg)
                        continue

                    m_h = alibi_slope(hd, H)
                    kms, kmsb = kms2[hb_], kmsb2[hb_]

                    def loadfn(hd=hd, hb_=hb_, kT=kT, kb=kb, kms=kms, kmsb=kmsb):
                        load_head(hd, H + hd, hb_)
                        S.op("dve", lambda e: e.tensor_reduce(out=kms[:], in_=kT[:].rearrange("p (n l) -> p n l", l=256),
                                                              axis=AX.X, op=ALU.add),
                             reads=[kb], writes=[kmsb])
                    storefn = (lambda hd=hd, ahi=ahi: S.op(
                        "sp", (lambda e: e.dma_start(out=attnT[hd, :, :], in_=ah[ahi][:])),
                        reads=[ahb[ahi]], dma=True))
                    for i in range(QT):
                        tc = tcount
                        tcount += 1
                        ei, wi, ti_, si, qi = tc % NE, tc % NW, tc % NWT, tc % NSM, tc % 2
                        vb = i
                        nk = (vb + 1) * 256
                        c0 = C0 - 256 * i

                        def m0p(i=i, qi=qi, qT=qT, qb=qb, kms=kms, kmsb=kmsb):
                            S.op("pool", (lambda e: e.tensor_copy(out=q32[qi][:], in_=qT[:, i * 128:(i + 1) * 128])),
                                 reads=[qb], writes=[q32b[qi]])
                            S.op("pe", (lambda e: e.matmul(gbk, lhsT=q32[qi][:], rhs=kms[:], start=True, stop=True)),
                                 reads=[q32b[qi], kmsb], writes=[gbkb])

                        def m0a(i=i, si=si, qi=qi, vb=vb):
                            S.op("dve", (lambda e: e.tensor_tensor(out=sm[si][:, 0:NBLK], in0=gbk,
                                                                   in1=mb[:, i, :], op=ALU.add)),
                                 reads=[gbkb, constb], writes=[smb[si]])
                            if NBLK < 8:
                                S.op("dve", (lambda e: e.memset(sm[si][:, NBLK:8], -3.0e38)),
                                     reads=[smb[si]], writes=[smb[si]])
                            S.op("dve", (lambda e: e.max(out=sm[si][:, 16:24], in_=sm[si][:, 0:max(NBLK, 8)])),
                                 reads=[smb[si]], writes=[smb[si]])
                            S.op("dve", (lambda e: e.tensor_scalar(
                                out=sm[si][:, 24:24 + NBLK], in0=sm[si][:, 0:NBLK], scalar1=sm[si][:, 18:19],
                                scalar2=BIG / scale, op0=ALU.is_ge, op1=ALU.mult)),
                                 reads=[smb[si]], writes=[smb[si]])
                            S.op("dve", (lambda e: e.scalar_tensor_tensor(
                                out=selb[qi][:], in0=sm[si][:, 24:24 + NBLK], scalar=-BIG / scale, in1=mb[:, i, :],
                                op0=ALU.add, op1=ALU.add)),
                                 reads=[smb[si], constb], writes=[selbb[qi]])
                            S.op("dve", (lambda e: e.memset(selb[qi][:, vb:vb + 1], 0.0)),
                                 reads=[selbb[qi]], writes=[selbb[qi]])

                        def m0b(qi=qi):
                            S.op("pe", (lambda e: e.transpose(msc2[0:NBLK, :], selb[qi][:], ident[:])),
                                 reads=[selbb[qi], constb], writes=[msc2b])
                            S.op("act", (lambda e: e.activation(out=selbT[qi][:], in_=msc2[0:NBLK, :], func=AF.Copy)),
                                 reads=[msc2b], writes=[selbTb[qi]])

                        def m1(i=i, ei=ei, qi=qi, nk=nk, c0=c0, m_h=m_h, qT=qT, kT=kT, qb=qb, kb=kb):
                            for c in range(-(-nk // 512)):
                                w = min(512, nk - 512 * c)
                                zi = nxt("z", NZ)

                                def zmm(e, zi=zi, c=c, w=w):
                                    e.matmul(zb[zi][:, 0:w], lhsT=qT[:, i * 128:(i + 1) * 128],
                                             rhs=kT[:, 512 * c:512 * c + w], start=True, stop=False)
                                    return e.matmul(zb[zi][:, 0:w], lhsT=selbT[qi][:],
                                                    rhs=ind[:, 512 * c:512 * c + w], start=False, stop=True)
                                S.op("pe", zmm, reads=[qb, kb, selbTb[qi], constb], writes=[zbb[zi]])
                                S.op("dve", (lambda e, zi=zi, c=c, w=w: e.scalar_tensor_tensor(
                                    out=E[ei][:, 512 * c:512 * c + w], in0=b0m[:, c0 + 512 * c:c0 + 512 * c + w],
                                    scalar=-m_h / scale, in1=zb[zi][:, 0:w], op0=ALU.mult, op1=ALU.add)),
                                     reads=[zbb[zi], constb], writes=[Eb[ei]])

                        def m2a(ei=ei, si=si, nk=nk):
                            S.op("dve", (lambda e: e.tensor_reduce(out=sm[si][:, 56:57], in_=E[ei][:, 0:nk],
                                                                   axis=AX.X, op=ALU.max)),
                                 reads=[Eb[ei]], writes=[smb[si]])
                            S.op("dve", (lambda e: e.tensor_scalar(out=sm[si][:, 57:58], in0=sm[si][:, 56:57],
                                                                   scalar1=-scale, scalar2=None, op0=ALU.mult)),
                                 reads=[smb[si]], writes=[smb[si]])

                        def m2b(ei=ei, wi=wi, si=si, nk=nk):
                            S.op("act", (lambda e: e.activation(
                                out=W[wi][:, 0:nk], in_=E[ei][:, 0:nk], func=AF.Exp, bias=sm[si][:, 57:58], scale=scale,
                                accum_out=sm[si][:, 58:59])),
                                 reads=[Eb[ei], smb[si]], writes=[Wb[wi], smb[si]])

                        def m3(i=i, wi=wi, ti_=ti_, si=si, qi=qi, nk=nk, ahi=ahi, V=V, Vb=Vb):
                            S.op("dve", (lambda e: e.reciprocal(out=sm[si][:, 59:60], in_=sm[si][:, 58:59])),
                                 reads=[smb[si]], writes=[smb[si]])
                            transposes(wi, ti_, nk)
                            nt = nk // 128

                            def pv(e):
                                ins = None
                                for j in range(nt):
                                    ins = e.matmul(ob, lhsT=WT[ti_][:, j, :], rhs=V[:, j, :],
                                                   start=(j == 0), stop=(j == nt - 1))
                                return ins
                            S.op("pe", pv, reads=[Vb, WTb[ti_]], writes=[obb])

                        def m4(si=si, qi=qi):
                            S.op("dve", (lambda e: e.tensor_scalar(out=osb[qi][:], in0=ob, scalar1=sm[si][:, 59:60],
                                                                   scalar2=None, op0=ALU.mult)),
                                 reads=[obb, smb[si]], writes=[osbb[qi]])
                            S.op("pe", (lambda e: e.transpose(msc, osb[qi][:], ident[:])),
                                 reads=[osbb[qi], constb], writes=[mscb])

                        def m5(i=i, ahi=ahi):
                            S.op("dve", (lambda e: e.tensor_copy(out=ah[ahi][:, i * 128:(i + 1) * 128], in_=msc)),
                                 reads=[mscb], writes=[ahb[ahi]])
                        stg = [(-1, 0, m0p), (0, 0, m0a), (1, 1, m1), (2, 2, m2a), (3, 3, m2b), (4, 4, m3),
                               (5, 3.5, m4), (6, 3.4, m5), (0, 8, m0b)]
                        if i == 0:
                            stg.append((-3, -1, loadfn))
                        if i == QT - 1:
                            stg.append((7, 9, storefn))
                        tiles.append(stg)
                pipeline(tiles)
                S.drain_dmas("sp")
                with nc.Block() as block:
                    S.emit(block)

        pieceb = [Buf("piece%d" % p_) for p_ in range(cfg.NP)]
        phase_ac("A")
        phase_b("sb", True)
        phase_b("moba", False)
        phase_ac("C")
    return nc


def tile_w(W):
    K, N = W.shape
    return np.ascontiguousarray(W.reshape(K // 128, 128, N // 128, 128).transpose(2, 1, 0, 3))


def fm(a):
    T, C = a.shape
    return np.ascontiguousarray(a.T.reshape(C // 128, 128, T).transpose(1, 0, 2))


def consts(cfg, hf):
    t = np.arange(128)[:, None]
    ident = (t == np.arange(128)[None, :]).astype(np.float32)
    strict = (np.arange(128)[None, :] < t).astype(np.float32)
    if hf == 0:
        mask2 = np.concatenate([strict, np.zeros((128, 128), np.float32)], axis=1)
    else:
        mask2 = np.concatenate([np.ones((128, 128), np.float32), strict], axis=1)
    mask2 = (mask2 - 1.0) * np.float32(BIG)
    c = np.arange(cfg.TABW)[None, :]
    d = (t + 128 * hf - (c - cfg.C0)).astype(np.float32)
    b0m = np.where(d >= 0, d, np.float32(1.0e9)).astype(np.float32)
    return ident, np.ascontiguousarray(mask2), b0m


def mask_bias(cfg):
    mbv = np.zeros((cfg.QT, cfg.NBLK), np.float32)
    for i in range(cfg.QT):
        mbv[i, i:] = -1.0e30
    return np.ascontiguousarray(np.broadcast_to(mbv[None], (128, cfg.QT, cfg.NBLK)))


def run(cfg, inp):
    D, SH = cfg.D, cfg.SH
    nc = build_program(cfg)
    f32 = np.float32
    wmap = {"f1g": "ffn1_w_gate", "f1u": "ffn1_w_up", "f1d": "ffn1_w_down", "win": "w_in", "wbm": "w_branch_moba",
            "wbs": "w_branch_sb", "wout": "w_out", "f2g": "ffn2_w_gate", "f2u": "ffn2_w_up", "f2d": "ffn2_w_down",
            "wpg": "w_ple_gate", "wpp": "w_ple_proj"}
    shared = {k: tile_w(np.asarray(inp[v], f32)[0]) for k, v in wmap.items()}
    g = np.stack([np.asarray(inp[n], f32).reshape(-1) for n in
                  ("ffn1_norm", "mix_norm", "ffn2_norm", "ple_norm", "final_norm")])
    shared["gall"] = np.ascontiguousarray(g.reshape(5, cfg.KC, 128).transpose(2, 0, 1))
    shared["cind"] = (np.arange(cfg.SV)[None, :] // 256 == np.arange(cfg.NBLK)[:, None]).astype(np.float32)
    shared["mbias"] = mask_bias(cfg)
    x = np.asarray(inp["x"], f32)
    p = np.asarray(inp["p"], f32)[0]
    nb = cfg.QT
    in_maps = []
    for r in range(cfg.NCORES):
        b, hf = r // 2, r % 2
        m = dict(shared)
        m["cident"], m["cmask2"], m["cb0m"] = consts(cfg, hf)
        m["xT"] = fm(x[b].reshape(nb, 2, 128, D)[:, hf].reshape(SH, D))
        m["pT"] = fm(p[b].reshape(nb, 2, 128, -1)[:, hf].reshape(SH, -1))
        in_maps.append(m)
    res = run_bass_kernel_spmd(nc, in_maps, core_ids=list(range(cfg.NCORES)))
    out = np.empty((cfg.B, 2 * SH, D), f32)
    for r in range(cfg.NCORES):
        b, hf = r // 2, r % 2
        oT = np.asarray(res.results[r]["outT"], f32)
        out[b].reshape(nb, 2, 128, D)[:, hf] = oT.transpose(2, 1, 0).reshape(nb, 128, D)
    return out


def kernel(**inputs):
    return run(FULL, inputs)
```

```python
import numpy as np
import ml_dtypes
from contextlib import ExitStack
import concourse.bass as bass
import concourse.mybir as mybir
from concourse.bass_utils import run_bass_kernel_spmd

F32 = mybir.dt.float32
BF16 = mybir.dt.bfloat16
AF = mybir.ActivationFunctionType
ALU = mybir.AluOpType
AX = mybir.AxisListType

RMS_EPS = 1e-6
BIG = 30000.0
NDS = 24
WK = 16


class Cfg:
    def __init__(self, D, F, SH, NT, H, B):
        self.D, self.F, self.SH, self.NT, self.H, self.B = D, F, SH, NT, H, B
        self.KC = D // 128
        self.FC = F // 128
        self.TT = SH // NT
        self.SV = 2 * SH
        self.QT = SH // 128
        self.NBLK = self.SV // 256
        self.NQKV = 6 * H
        self.INC = 6 * H * 128 + 2 * D
        self.C0 = 256 * (self.QT - 1)
        self.TABW = 256 * self.QT
        self.NP = (4 * H) // 4
        self.GC = min(32, self.FC)
        self.NCORES = 2 * B


FULL = Cfg(D=4096, F=11008, SH=2048, NT=512, H=16, B=4)


class Buf:
    __slots__ = ("name", "w", "r")

    def __init__(self, name):
        self.name = name
        self.w = None
        self.r = {}


class Sched:
    def __init__(self, nc, es):
        self.nc = nc
        self.eng = {"pe": nc.tensor, "act": nc.scalar, "dve": nc.vector, "pool": nc.gpsimd, "sp": nc.sync}
        self.sem = {k: es.enter_context(nc.semaphore("s_" + k)) for k in self.eng}
        self.cnt = {k: 0 for k in self.eng}
        self.dsem = [es.enter_context(nc.semaphore("d%d" % i)) for i in range(NDS)]
        self.dcnt = [0] * NDS
        self.dnext = {"sp": 0, "pool": 0, "act": 0}
        self.dbase = {"sp": 0, "pool": NDS // 2, "act": 0}
        self.ops = {k: [] for k in self.eng}
        self.waited = {k: {} for k in self.eng}
        self.csem = []
        self.es = es

    def op(self, e, fn, reads=(), writes=(), dma=False):
        deps = []
        for b in reads:
            if b.w is not None:
                deps.append(b.w)
        for b in writes:
            if b.w is not None:
                deps.append(b.w)
            for k, v in b.r.items():
                deps.append((k, v))
        if dma:
            i = self.dbase[e] + self.dnext[e]
            self.dnext[e] = (self.dnext[e] + 1) % (NDS // 2)
            if self.dcnt[i] > 0:
                deps.append((("d", i), self.dcnt[i]))
            self.dcnt[i] += 16
            ev = (("d", i), self.dcnt[i])
        else:
            self.cnt[e] += 1
            ev = (("e", e), self.cnt[e])
        waits = {}
        wd = self.waited[e]
        for key, val in deps:
            if key == ("e", "pe") and e == "pe" and not dma:
                continue
            if wd.get(key, 0) >= val:
                continue
            if waits.get(key, 0) < val:
                waits[key] = val
        for k, v in waits.items():
            wd[k] = v
        self.ops[e].append((list(waits.items()), fn, ev))
        for b in reads:
            if b.r.get(ev[0], 0) < ev[1]:
                b.r[ev[0]] = ev[1]
        for b in writes:
            b.w = ev
            b.r = {}
        return ev

    def cc_op(self, fn, writes):
        i = len(self.csem)
        self.csem.append(self.es.enter_context(self.nc.semaphore("cc%d" % i)))
        ev = (("c", i), 1)
        self.ops["pool"].append(([], fn, ev))
        for b in writes:
            b.w = ev
            b.r = {}
        return ev

    def drain_dmas(self, e="sp"):
        waits = []
        for i in range(NDS):
            if self.dcnt[i] > self.waited[e].get(("d", i), 0):
                waits.append((("d", i), self.dcnt[i]))
                self.waited[e][("d", i)] = self.dcnt[i]
        self.ops[e].append((waits, None, None))

    def _semof(self, key):
        if key[0] == "c":
            return self.csem[key[1]]
        return self.dsem[key[1]] if key[0] == "d" else self.sem[key[1]]

    def emit(self, block):
        def run(name):
            def body(engine):
                for waits, fn, ev in self.ops[name]:
                    for key, val in waits:
                        engine.wait_ge(self._semof(key), val)
                    if fn is None:
                        continue
                    inst = fn(engine)
                    if ev[0][0] == "c":
                        inst.then_inc(self._semof(ev[0]))
                    else:
                        inst.then_inc(self._semof(ev[0]), 16 if ev[0][0] == "d" else 1)
            return body
        for name, dec in (("sp", block.sync), ("pe", block.tensor), ("act", block.scalar),
                          ("dve", block.vector), ("pool", block.gpsimd)):
            if self.ops[name]:
                dec(run(name))
            self.ops[name] = []


def alibi_slope(h, H):
    return float(2.0 ** (-8.0 * (h + 1) / H))


def build_program(cfg):
    D, F, SH, NT, H = cfg.D, cfg.F, cfg.SH, cfg.NT, cfg.H
    KC, FC, TT, SV, QT, NBLK = cfg.KC, cfg.FC, cfg.TT, cfg.SV, cfg.QT, cfg.NBLK
    C0, TABW, GC = cfg.C0, cfg.TABW, cfg.GC
    scale = 128.0 ** -0.5

    nc = bass.Bass("TRN2", target_bir_lowering=False)

    def din(name, shape, dt=F32):
        return nc.dram_tensor(name, list(shape), dt, kind="ExternalInput").ap()

    xT = din("xT", [128, KC, SH])
    pT = din("pT", [128, 2, SH])
    gall = din("gall", [128, 5, KC])
    mbias = din("mbias", [128, QT, NBLK])
    cid = din("cident", [128, 128])
    cms = din("cmask2", [128, 256])
    cb0 = din("cb0m", [128, TABW])
    cind = din("cind", [NBLK, SV])
    Wd_ = {}
    for nm, K, N in (("f1g", D, F), ("f1u", D, F), ("f1d", F, D), ("win", D, cfg.INC), ("wbm", H * 128, D),
                     ("wbs", H * 128, D), ("wout", D, D), ("f2g", D, F), ("f2u", D, F), ("f2d", F, D),
                     ("wpg", D, D), ("wpp", 256, D)):
        Wd_[nm] = din(nm, [N // 128, 128, K // 128, 128])
    outT = nc.dram_tensor("outT", [128, KC, SH], F32, kind="ExternalOutput").ap()
    h1 = nc.dram_tensor("h1s", [128, KC, SH], F32, kind="Internal").ap()
    qTs = nc.dram_tensor("qTs", [2 * H, 128, SH], BF16, kind="Internal").ap()
    kvloc_t = nc.dram_tensor("kvloc", [4 * H * 128, SH], BF16)
    kvloc = kvloc_t.ap()
    kvall_t = [nc.dram_tensor("kvall%d" % p_, [2 * 512, SH], BF16) for p_ in range(cfg.NP)]
    kvall = [t_.ap() for t_ in kvall_t]
    groups = [[2 * b_, 2 * b_ + 1] for b_ in range(cfg.B)]

    def kv_slot(j):
        if 4 * H <= j < 5 * H:
            return 2 * (j - 4 * H)
        if 5 * H <= j < 6 * H:
            return 2 * (j - 5 * H) + 1
        if H <= j < 2 * H:
            return 2 * (H + j - H)
        return 2 * (H + j - 2 * H) + 1
    attnT = nc.dram_tensor("attns", [2 * H, 128, SH], BF16, kind="Internal").ap()

    with ExitStack() as es0:
        S = Sched(nc, es0)
        cnt = [0]

        def sb(es, shape, dt, nm=None):
            cnt[0] += 1
            return es.enter_context(nc.sbuf_tensor("%s_%d" % (nm or "t", cnt[0]), list(shape), dt))

        def ps(es, shape, dt, nm=None):
            cnt[0] += 1
            return es.enter_context(nc.psum_tensor("%s_%d" % (nm or "p", cnt[0]), list(shape), dt))

        def phase_ac(which):
            with ExitStack() as es:
                h = sb(es, [128, KC, NT], F32, "h")
                xn = sb(es, [128, KC, NT], BF16, "xn")
                big2 = sb(es, [128, max(GC, 2 * H), NT], BF16, "big2")
                gated = sb(es, [128, KC, NT], BF16, "gated") if which == "C" else None
                NWS = 6
                wring = [sb(es, [128, WK, 128], BF16, "w") for _ in range(NWS)]
                gsb = sb(es, [128, 5, KC], F32, "g")
                ones32 = sb(es, [128, 128], F32, "ones")
                sq = [sb(es, [128, NT], F32, "sq") for _ in range(2)]
                rs = sb(es, [128, NT], F32, "rs")
                acc = sb(es, [128, NT], F32, "acc")
                rs2 = sb(es, [128, NT], F32, "rs2")
                tmp = [sb(es, [128, NT], F32, "tmp") for _ in range(3)]
                stage = [sb(es, [128, NT], BF16, "st") for _ in range(4)]
                pbf = sb(es, [128, 2, NT], BF16, "pbf")
                banksF = [ps(es, [128, 512], F32, "bk") for _ in range(8)]
                banks = [b[:, 0:NT] for b in banksF]

                hb = [Buf("h%d" % c) for c in range(KC)]
                xnb = [Buf("xn%d" % c) for c in range(KC)]
                b2b = [Buf("b2_%d" % c) for c in range(max(GC, 2 * H))]
                gtb = [Buf("gt%d" % c) for c in range(KC)]
                wrb = [Buf("w%d" % i) for i in range(NWS)]
                sqb = [Buf("sq0"), Buf("sq1")]
                rsb, rs2b, gb, onesb, pbfb = Buf("rs"), Buf("rs2"), Buf("g"), Buf("ones"), Buf("pbf")
                accb = Buf("acc")
                tmpb = [Buf("tmp%d" % i) for i in range(4)]
                stb = [Buf("st%d" % i) for i in range(4)]
                bkb = [Buf("bk%d" % i) for i in range(8)]
                st = {"w": 0, "tmp": 0, "st": 0, "bk": {}}

                S.op("sp", lambda e: e.dma_start(out=gsb[:], in_=gall), writes=[gb], dma=True)
                S.op("dve", lambda e: e.memset(ones32[:], 1.0), writes=[onesb])

                def mm_group(bank_i, Wt, j, k0, k1, rhs_fn, rhs_bufs):
                    k = k0
                    while k < k1:
                        kk = min(k + WK, k1)
                        si = st["w"] % NWS
                        st["w"] += 1
                        wt, wb = wring[si], wrb[si]
                        S.op("pool", (lambda e, wt=wt, k=k, kk=kk: e.dma_start(
                            out=wt[:, 0:kk - k, :], in_=Wt[j, :, k:kk, :])), writes=[wb], dma=True)

                        def mm(e, wt=wt, k=k, kk=kk):
                            ins = None
                            for q in range(k, kk):
                                ins = e.matmul(banks[bank_i], lhsT=wt[:, q - k, :], rhs=rhs_fn(q),
                                               start=(q == k0), stop=(q == k1 - 1))
                            return ins
                        S.op("pe", mm, reads=[wb] + [rhs_bufs[q] for q in range(k, kk)], writes=[bkb[bank_i]])
                        k = kk

                def nxt(kind, n):
                    i = st[kind] % n
                    st[kind] += 1
                    return i

                def stats_chunk(c):
                    if c == 0:
                        S.op("act", (lambda e: e.activation(out=acc[:], in_=h[:, 0, :], func=AF.Square)),
                             reads=[hb[0]], writes=[accb])
                        return
                    s_ = c % 2
                    S.op("act", (lambda e, c=c, s_=s_: e.activation(out=sq[s_][:], in_=h[:, c, :], func=AF.Square)),
                         reads=[hb[c]], writes=[sqb[s_]])
                    S.op("dve", (lambda e, s_=s_: e.tensor_tensor(out=acc[:], in0=acc[:], in1=sq[s_][:], op=ALU.add)),
                         reads=[sqb[s_], accb], writes=[accb])

                def stats_all():
                    for c in range(KC):
                        stats_chunk(c)

                def norm(gidx, out_t, out_b):
                    NB = 6
                    S.op("pe", (lambda e: e.matmul(banks[NB], lhsT=ones32[:], rhs=acc[:], start=True, stop=True)),
                         reads=[accb, onesb], writes=[bkb[NB]])
                    S.op("act", lambda e: e.activation(out=rs[:], in_=banks[NB], func=AF.Sqrt,
                                                       bias=RMS_EPS, scale=1.0 / D),
                         reads=[bkb[NB]], writes=[rsb])
                    S.op("dve", lambda e: e.reciprocal(out=rs2[:], in_=rs[:]), reads=[rsb], writes=[rs2b])
                    for c in range(KC):
                        S.op("dve", (lambda e, c=c: e.scalar_tensor_tensor(
                            out=out_t[:, c, :], in0=h[:, c, :], scalar=gsb[:, gidx, c:c + 1], in1=rs2[:],
                            op0=ALU.mult, op1=ALU.mult)),
                             reads=[hb[c], gb, rs2b], writes=[out_b[c]])

                def ffn(wg, wu, wdn, gidx):
                    norm(gidx, xn, xnb)
                    f0 = 0
                    ng = -(-FC // GC)
                    gs = -(-FC // ng)
                    while f0 < FC:
                        f1 = min(FC, f0 + gs)
                        for f in range(f0, f1):
                            bg = (f % 2)
                            bu = 2 + (f % 2)
                            mm_group(bg, wg, f, 0, KC, lambda q: xn[:, q, :], xnb)
                            mm_group(bu, wu, f, 0, KC, lambda q: xn[:, q, :], xnb)
                            ti = nxt("tmp", 3)
                            S.op("act", (lambda e, bg=bg, ti=ti: e.activation(out=tmp[ti][:], in_=banks[bg],
                                                                            func=AF.Silu)),
                                 reads=[bkb[bg]], writes=[tmpb[ti]])
                            S.op("dve", (lambda e, bu=bu, ti=ti, f=f, f0=f0: e.tensor_tensor(
                                out=big2[:, f - f0, :], in0=banks[bu], in1=tmp[ti][:], op=ALU.mult)),
                                 reads=[bkb[bu], tmpb[ti]], writes=[b2b[f - f0]])
                        for c in range(KC):
                            bd = 4 + (c % 2)
                            mm_group(bd, wdn, c, f0, f1, (lambda q, f0=f0: big2[:, q - f0, :]),
                                     {q: b2b[q - f0] for q in range(f0, f1)})
                            S.op("dve", (lambda e, bd=bd, c=c: e.scalar_tensor_tensor(
                                out=h[:, c, :], in0=banks[bd], scalar=0.5, in1=h[:, c, :],
                                op0=ALU.mult, op1=ALU.add)),
                                 reads=[bkb[bd], hb[c]], writes=[hb[c]])
                            if f1 == FC:
                                stats_chunk(c)
                        f0 = f1

                if which == "A":
                    for vt in range(TT):
                        own = True
                        t0 = vt * NT
                        S.op("sp", (lambda e, t0=t0: e.dma_start(out=h[:], in_=xT[:, :, t0:t0 + NT])),
                             writes=hb, dma=True)
                        stats_all()
                        ffn(Wd_["f1g"], Wd_["f1u"], Wd_["f1d"], 0)
                        if own:
                            S.op("sp", (lambda e, t0=t0: e.dma_start(out=h1[:, :, t0:t0 + NT], in_=h[:])),
                                 reads=hb, dma=True)
                        norm(1, xn, xnb)
                        chunks = list(range(6 * H)) if own else (list(range(H, 3 * H)) + list(range(4 * H, 6 * H)))
                        for n_, j in enumerate(chunks):
                            bk = n_ % 4
                            mm_group(bk, Wd_["win"], j, 0, KC, lambda q: xn[:, q, :], xnb)
                            si = nxt("st", 4)
                            if n_ % 2 == 0:
                                S.op("act", (lambda e, bk=bk, si=si: e.activation(out=stage[si][:], in_=banks[bk],
                                                                                func=AF.Copy)),
                                     reads=[bkb[bk]], writes=[stb[si]])
                            else:
                                S.op("dve", (lambda e, bk=bk, si=si: e.tensor_copy(out=stage[si][:], in_=banks[bk])),
                                     reads=[bkb[bk]], writes=[stb[si]])
                            if j < H or 3 * H <= j < 4 * H:
                                dst = qTs[j if j < H else j - 2 * H, :, t0:t0 + NT]
                            else:
                                sl = kv_slot(j)
                                dst = kvloc[sl * 128:(sl + 1) * 128, t0:t0 + NT]
                            S.op("sp", (lambda e, dst=dst, si=si: e.dma_start(out=dst, in_=stage[si][:])),
                                 reads=[stb[si]], dma=True)
                else:
                    for t in range(TT):
                        t0 = t * NT
                        S.op("sp", (lambda e, t0=t0: e.dma_start(out=h[:], in_=h1[:, :, t0:t0 + NT])),
                             writes=hb, dma=True)
                        stats_all()
                        S.op("sp", (lambda e, t0=t0: e.dma_start(
                            out=big2[:, 0:2 * H, :], in_=attnT[:, :, t0:t0 + NT].rearrange("j p t -> p j t"))),
                             writes=b2b[0:2 * H], dma=True)
                        S.op("pool", (lambda e, t0=t0: e.dma_start(out=pbf[:], in_=pT[:, :, t0:t0 + NT])),
                             writes=[pbfb], dma=True)
                        norm(1, xn, xnb)
                        for j in range(KC):
                            mm_group(0, Wd_["win"], 6 * H + j, 0, KC, lambda q: xn[:, q, :], xnb)
                            mm_group(1, Wd_["win"], 6 * H + KC + j, 0, KC, lambda q: xn[:, q, :], xnb)
                            mm_group(2, Wd_["wbm"], j, 0, H, lambda q: big2[:, q, :], b2b)
                            mm_group(3, Wd_["wbs"], j, 0, H, lambda q: big2[:, H + q, :],
                                     {q: b2b[H + q] for q in range(H)})
                            ta, tb = nxt("tmp", 3), nxt("tmp", 3)
                            S.op("act", (lambda e, ta=ta: e.activation(out=tmp[ta][:], in_=banks[0], func=AF.Sigmoid)),
                                 reads=[bkb[0]], writes=[tmpb[ta]])
                            S.op("act", (lambda e, tb=tb: e.activation(out=tmp[tb][:], in_=banks[1], func=AF.Sigmoid)),
                                 reads=[bkb[1]], writes=[tmpb[tb]])
                            S.op("dve", (lambda e, ta=ta: e.tensor_tensor(out=tmp[ta][:], in0=banks[2], in1=tmp[ta][:],
                                                                         op=ALU.mult)),
                                 reads=[bkb[2], tmpb[ta]], writes=[tmpb[ta]])
                            S.op("dve", (lambda e, tb=tb: e.tensor_tensor(out=tmp[tb][:], in0=banks[3], in1=tmp[tb][:],
                                                                         op=ALU.mult)),
                                 reads=[bkb[3], tmpb[tb]], writes=[tmpb[tb]])
                            S.op("dve", (lambda e, ta=ta, tb=tb, j=j: e.tensor_tensor(
                                out=gated[:, j, :], in0=tmp[ta][:], in1=tmp[tb][:], op=ALU.add)),
                                 reads=[tmpb[ta], tmpb[tb]], writes=[gtb[j]])
                        for c in range(KC):
                            bd = 4 + (c % 2)
                            mm_group(bd, Wd_["wout"], c, 0, KC, lambda q: gated[:, q, :], gtb)
                            S.op("dve", (lambda e, bd=bd, c=c: e.tensor_tensor(out=h[:, c, :], in0=banks[bd],
                                                                             in1=h[:, c, :], op=ALU.add)),
                                 reads=[bkb[bd], hb[c]], writes=[hb[c]])
                            stats_chunk(c)
                        ffn(Wd_["f2g"], Wd_["f2u"], Wd_["f2d"], 2)
                        norm(3, xn, xnb)
                        for c in range(KC):
                            bg, bu = (c % 2), 2 + (c % 2)
                            mm_group(bg, Wd_["wpg"], c, 0, KC, lambda q: xn[:, q, :], xnb)
                            mm_group(bu, Wd_["wpp"], c, 0, 2, lambda q: pbf[:, q, :], {0: pbfb, 1: pbfb})
                            ti = nxt("tmp", 3)
                            S.op("act", (lambda e, bg=bg, ti=ti: e.activation(out=tmp[ti][:], in_=banks[bg],
                                                                            func=AF.Sigmoid)),
                                 reads=[bkb[bg]], writes=[tmpb[ti]])
                            S.op("dve", (lambda e, bu=bu, ti=ti: e.tensor_tensor(out=tmp[ti][:], in0=banks[bu],
                                                                               in1=tmp[ti][:], op=ALU.mult)),
                                 reads=[bkb[bu], tmpb[ti]], writes=[tmpb[ti]])
                            S.op("dve", (lambda e, ti=ti, c=c: e.tensor_tensor(out=h[:, c, :], in0=h[:, c, :],
                                                                              in1=tmp[ti][:], op=ALU.add)),
                                 reads=[tmpb[ti], hb[c]], writes=[hb[c]])
                            stats_chunk(c)
                        norm(4, h, hb)
                        S.op("sp", (lambda e, t0=t0: e.dma_start(out=outT[:, :, t0:t0 + NT], in_=h[:])),
                             reads=hb, dma=True)
                S.drain_dmas("sp")
                with nc.Block() as block:
                    S.emit(block)

        NBUF = 3

        def phase_b(kind, first):
            with ExitStack() as es:
                is_sb = kind == "sb"
                ident = sb(es, [128, 128], BF16, "ident")
                qT2 = [sb(es, [128, SH], BF16, "qT") for _ in range(2)]
                kT2 = [sb(es, [128, SV], BF16, "kT") for _ in range(2)]
                vT = sb(es, [128, SV], BF16, "vT")
                V2 = [sb(es, [128, SV // 128, 128], BF16, "V") for _ in range(2)]
                ah = [sb(es, [128, SH], BF16, "ah") for _ in range(2)]
                NE = 3
                NW, NWT, NSM, NLP = 3, 2, 8, 3
                E = [sb(es, [128, SV], F32, "E") for _ in range(NE)]
                W = [sb(es, [128, SV], BF16, "W") for _ in range(NW)]
                WT = [sb(es, [128, SV // 128, 128], BF16, "WT") for _ in range(NWT)]
                sm = [sb(es, [128, 64], F32, "sm") for _ in range(NSM)]
                if is_sb:
                    negm = sb(es, [128, 256], BF16, "negm")
                    ones_bf = sb(es, [128, SV], BF16, "onesbf")
                    LP = [sb(es, [128, SV + 1], F32, "LP") for _ in range(NLP)]
                else:
                    b0m = sb(es, [128, TABW], F32, "b0m")
                    mb = sb(es, [128, QT, NBLK], F32, "mb")
                    ind = sb(es, [NBLK, SV], BF16, "ind")
                    q32 = [sb(es, [128, 128], F32, "q32") for _ in range(NBUF)]
                    kms2 = [sb(es, [128, NBLK], F32, "kms") for _ in range(2)]
                    selb = [sb(es, [128, NBLK], BF16, "selb") for _ in range(NBUF)]
                    selbT = [sb(es, [NBLK, 128], BF16, "selbT") for _ in range(NBUF)]
                    osb = [sb(es, [128, 128], BF16, "osb") for _ in range(NBUF)]
                NZ = 2
                zb = [ps(es, [128, 512], F32, "z") for _ in range(NZ)]
                msc2F = ps(es, [128, 8, 128], BF16, "msc2")
                msc2 = msc2F[:, 0, :]
                trbF = [ps(es, [128, 8, 128], BF16, "tr") for _ in range(2)]
                trb = [t[:, 0:4, :] for t in trbF]
                obF = ps(es, [128, 512], F32, "o")
                ob = obF[:, 0:128]
                gbkF = ps(es, [128, 512], F32, "gt")
                gbk = gbkF[:, 0:NBLK]
                mscF = ps(es, [128, 8, 128], BF16, "msc")
                msc = mscF[:, 0, :]

                constb, vb_ = Buf("const"), Buf("v")
                qb2, kb2, Vb2, kmsb2 = ([Buf(n + str(i)) for i in range(2)] for n in ("q", "k", "V", "kms"))
                ahb = [Buf("ah0"), Buf("ah1")]
                Eb, LPb, Wb, WTb, smb, selbb, selbTb, osbb, q32b = (
                    [Buf(n + str(i)) for i in range(8)]
                    for n in ("E", "LP", "W", "WT", "sm", "selb", "selbT", "osb", "q32"))
                zbb = [Buf("z%d" % i) for i in range(NZ)]
                msc2b = Buf("msc2")
                trbb = [Buf("tr%d" % i) for i in range(2)]
                obb, gbkb, mscb = Buf("o"), Buf("gbk"), Buf("msc")
                st = {"z": 0, "tr": 0, "ev": 0}

                def nxt(kind_, n):
                    i = st[kind_] % n
                    st[kind_] += 1
                    return i

                if first:
                    for p_ in range(cfg.NP):
                        S.cc_op((lambda e, p_=p_: e.collective_compute(
                            "AllGather", ALU.bypass, replica_groups=groups,
                            ins=[kvloc_t.ap()[p_ * 512:(p_ + 1) * 512, :].opt()], outs=[kvall_t[p_].ap().opt()])),
                            writes=[pieceb[p_]])
                S.op("pool", lambda e: e.dma_start(out=ident[:], in_=cid), writes=[constb], dma=True)
                if is_sb:
                    S.op("pool", lambda e: e.dma_start(out=negm[:], in_=cms), writes=[constb], dma=True)
                    S.op("dve", lambda e: e.memset(ones_bf[:], 1.0), writes=[constb])
                    for i in range(NLP):
                        S.op("dve", (lambda e, i=i: e.memset(LP[i][:, 0:1], 0.0)), writes=[LPb[i]])
                else:
                    S.op("pool", lambda e: e.dma_start(out=ind[:], in_=cind), writes=[constb], dma=True)
                    S.op("sp", lambda e: e.dma_start(out=b0m[:], in_=cb0), writes=[constb], dma=True)
                    S.op("sp", lambda e: e.dma_start(out=mb[:], in_=mbias), writes=[constb], dma=True)

                def evac(fn_act, fn_dve, reads, writes, alt=False):
                    if ((not is_sb) and not alt) or nxt("ev", 2) == 0:
                        S.op("act", fn_act, reads=reads, writes=writes)
                    else:
                        S.op("dve", fn_dve, reads=reads, writes=writes)

                def load_head(qidx, oi, hb_):
                    pc = oi // 2
                    qT, kT, V = qT2[hb_], kT2[hb_], V2[hb_]
                    qb, kb, Vb = qb2[hb_], kb2[hb_], Vb2[hb_]
                    S.op("sp", lambda e: e.dma_start(out=qT[:], in_=qTs[qidx, :, :]), writes=[qb], dma=True)
                    for kv, dstt, dstb in ((0, kT, kb), (1, vT, vb_)):
                        ro = ((2 * oi + kv) % 4) * 128
                        for r_ in range(2):
                            S.op("sp", (lambda e, dstt=dstt, ro=ro, r_=r_: e.dma_start(
                                out=dstt[:].rearrange("p (i r c) -> p i r c", r=2, c=128)[:, :, r_, :],
                                in_=kvall[pc][r_ * 512 + ro:r_ * 512 + ro + 128, :].rearrange("p (i c) -> p i c", c=128))),
                                 reads=[pieceb[pc]], writes=[dstb], dma=True)
                    for g in range(SV // 512):
                        ti = nxt("tr", 2)

                        def tr(e, g=g, ti=ti):
                            ins = None
                            for jj in range(4):
                                j = 4 * g + jj
                                ins = e.transpose(trb[ti][:, jj, :], vT[:, j * 128:(j + 1) * 128], ident[:])
                            return ins
                        S.op("pe", tr, reads=[vb_, constb], writes=[trbb[ti]])
                        evac((lambda e, g=g, ti=ti: e.activation(out=V[:, 4 * g:4 * g + 4, :], in_=trb[ti], func=AF.Copy)),
                             (lambda e, g=g, ti=ti: e.tensor_copy(out=V[:, 4 * g:4 * g + 4, :], in_=trb[ti])),
                             [trbb[ti]], [Vb], alt=True)

                def transposes(wi, ti_, n):
                    nt = n // 128
                    for g in range(-(-nt // 4)):
                        ti = nxt("tr", 2)
                        jn = min(4, nt - 4 * g)

                        def tr(e, g=g, ti=ti, jn=jn):
                            ins = None
                            for jj in range(jn):
                                j = 4 * g + jj
                                ins = e.transpose(trb[ti][:, jj, :], W[wi][:, j * 128:(j + 1) * 128], ident[:])
                            return ins
                        S.op("pe", tr, reads=[Wb[wi], constb], writes=[trbb[ti]])
                        evac((lambda e, g=g, ti=ti, jn=jn: e.activation(
                            out=WT[ti_][:, 4 * g:4 * g + jn, :], in_=trb[ti][:, 0:jn, :], func=AF.Copy)),
                             (lambda e, g=g, ti=ti, jn=jn: e.tensor_copy(
                                 out=WT[ti_][:, 4 * g:4 * g + jn, :], in_=trb[ti][:, 0:jn, :])),
                             [trbb[ti]], [WTb[ti_]])

                def pipeline(stage_lists):
                    bystep = {}
                    for i, x in enumerate(stage_lists):
                        for (l, o, f) in x:
                            bystep.setdefault(i + l, []).append((o, i, f))
                    for step in sorted(bystep):
                        for o, i, f in sorted(bystep[step], key=lambda z: (z[0], z[1])):
                            f()

                tcount = 0
                tiles = []
                for hd in range(H):
                    ahi = hd % 2
                    hb_ = hd % 2
                    qT, kT, V = qT2[hb_], kT2[hb_], V2[hb_]
                    qb, kb, Vb = qb2[hb_], kb2[hb_], Vb2[hb_]
                    if is_sb:
                        loadfn = (lambda hd=hd, hb_=hb_: load_head(H + hd, hd, hb_))
                        storefn = (lambda hd=hd, ahi=ahi: S.op(
                            "sp", (lambda e: e.dma_start(out=attnT[H + hd, :, :], in_=ah[ahi][:])),
                            reads=[ahb[ahi]], dma=True))
                        for i in range(QT):
                            tc = tcount
                            tcount += 1
                            ei, li, wi, ti_, si = tc % NE, tc % NLP, tc % NW, tc % NWT, tc % NSM
                            n = 256 * (i + 1)

                            def s0(i=i, ei=ei, li=li, n=n, qT=qT, kT=kT, qb=qb, kb=kb):
                                nch = -(-n // 512)
                                for c in range(nch):
                                    w = min(512, n - 512 * c)
                                    zi = nxt("z", NZ)
                                    last = c == nch - 1

                                    def zmm(e, zi=zi, c=c, w=w, last=last):
                                        ins = e.matmul(zb[zi][:, 0:w], lhsT=qT[:, i * 128:(i + 1) * 128],
                                                       rhs=kT[:, 512 * c:512 * c + w], start=True, stop=not last)
                                        if last:
                                            ins = e.matmul(zb[zi][:, w - 256:w], lhsT=ident[:], rhs=negm[:],
                                                           start=False, stop=True)
                                        return ins
                                    S.op("pe", zmm, reads=[qb, kb, constb], writes=[zbb[zi]])
                                    S.op("act", (lambda e, zi=zi, c=c, w=w: e.activation(
                                        out=E[ei][:, 512 * c:512 * c + w], in_=zb[zi][:, 0:w], func=AF.Exp, scale=scale)),
                                         reads=[zbb[zi]], writes=[Eb[ei]])
                                S.op("act", (lambda e: e.activation(out=LP[li][:, 1:n + 1], in_=E[ei][:, 0:n],
                                                                    func=AF.Ln, bias=1.0, scale=1.0)),
                                     reads=[Eb[ei]], writes=[LPb[li]])

                            def s1(li=li, si=si, n=n):
                                S.op("dve", (lambda e: e.tensor_tensor_scan(
                                    out=LP[li][:, 1:n + 1], data0=ones_bf[:, 0:n], data1=LP[li][:, 1:n + 1], initial=0.0,
                                    op0=ALU.mult, op1=ALU.add)),
                                     reads=[LPb[li], constb], writes=[LPb[li]])
                                S.op("dve", (lambda e: e.tensor_scalar(
                                    out=sm[si][:, 0:1], in0=LP[li][:, n:n + 1], scalar1=-1.0, scalar2=None, op0=ALU.mult)),
                                     reads=[LPb[li]], writes=[smb[si]])

                            def s2(li=li, wi=wi, si=si, n=n):
                                S.op("act", (lambda e: e.activation(out=W[wi][:, 0:n], in_=LP[li][:, 0:n],
                                                                    func=AF.Exp, bias=sm[si][:, 0:1], scale=1.0)),
                                     reads=[LPb[li], smb[si]], writes=[Wb[wi]])

                            def s3(ei=ei, wi=wi, n=n):
                                S.op("dve", (lambda e: e.tensor_tensor(
                                    out=W[wi][:, 0:n], in0=E[ei][:, 0:n], in1=W[wi][:, 0:n], op=ALU.mult)),
                                     reads=[Eb[ei], Wb[wi]], writes=[Wb[wi]])

                            def s4(i=i, wi=wi, ti_=ti_, n=n, ahi=ahi, V=V, Vb=Vb):
                                transposes(wi, ti_, n)
                                nt = n // 128

                                def pv(e):
                                    ins = None
                                    for j in range(nt):
                                        ins = e.matmul(ob, lhsT=V[:, j, :], rhs=WT[ti_][:, j, :],
                                                       start=(j == 0), stop=(j == nt - 1))
                                    return ins
                                S.op("pe", pv, reads=[Vb, WTb[ti_]], writes=[obb])
                                S.op("act", (lambda e: e.activation(out=ah[ahi][:, i * 128:(i + 1) * 128], in_=ob,
                                                                    func=AF.Copy)),
                                     reads=[obb], writes=[ahb[ahi]])
                            stg = [(0, 3, s0), (1, 2, s1), (2, 0, s2), (3, 1, s3), (4, 4, s4)]
                            if i == 0:
                                stg.append((-2, -1, loadfn))
                            if i == QT - 1:
                                stg.append((5, 9, storefn))
                            tiles.append(stg)
                        continue

                    m_h = alibi_slope(hd, H)
                    kms, kmsb = kms2[hb_], kmsb2[hb_]

                    def loadfn(hd=hd, hb_=hb_, kT=kT, kb=kb, kms=kms, kmsb=kmsb):
                        load_head(hd, H + hd, hb_)
                        S.op("dve", lambda e: e.tensor_reduce(out=kms[:], in_=kT[:].rearrange("p (n l) -> p n l", l=256),
                                                              axis=AX.X, op=ALU.add),
                             reads=[kb], writes=[kmsb])
                    storefn = (lambda hd=hd, ahi=ahi: S.op(
                        "sp", (lambda e: e.dma_start(out=attnT[hd, :, :], in_=ah[ahi][:])),
                        reads=[ahb[ahi]], dma=True))
                    for i in range(QT):
                        tc = tcount
                        tcount += 1
                        ei, wi, ti_, si, qi = tc % NE, tc % NW, tc % NWT, tc % NSM, tc % 3
                        vb = i
                        nk = (vb + 1) * 256
                        c0 = C0 - 256 * i

                        def m0p(i=i, qi=qi, qT=qT, qb=qb, kms=kms, kmsb=kmsb):
                            S.op("pool", (lambda e: e.tensor_copy(out=q32[qi][:], in_=qT[:, i * 128:(i + 1) * 128])),
                                 reads=[qb], writes=[q32b[qi]])
                            S.op("pe", (lambda e: e.matmul(gbk, lhsT=q32[qi][:], rhs=kms[:], start=True, stop=True)),
                                 reads=[q32b[qi], kmsb], writes=[gbkb])

                        def m0a(i=i, si=si, qi=qi, vb=vb):
                            S.op("dve", (lambda e: e.tensor_tensor(out=sm[si][:, 0:NBLK], in0=gbk,
                                                                   in1=mb[:, i, :], op=ALU.add)),
                                 reads=[gbkb, constb], writes=[smb[si]])
                            if NBLK < 8:
                                S.op("dve", (lambda e: e.memset(sm[si][:, NBLK:8], -3.0e38)),
                                     reads=[smb[si]], writes=[smb[si]])
                            S.op("dve", (lambda e: e.max(out=sm[si][:, 16:24], in_=sm[si][:, 0:max(NBLK, 8)])),
                                 reads=[smb[si]], writes=[smb[si]])
                            S.op("dve", (lambda e: e.tensor_scalar(
                                out=sm[si][:, 24:24 + NBLK], in0=sm[si][:, 0:NBLK], scalar1=sm[si][:, 18:19],
                                scalar2=BIG / scale, op0=ALU.is_ge, op1=ALU.mult)),
                                 reads=[smb[si]], writes=[smb[si]])
                            S.op("dve", (lambda e: e.scalar_tensor_tensor(
                                out=selb[qi][:], in0=sm[si][:, 24:24 + NBLK], scalar=-BIG / scale, in1=mb[:, i, :],
                                op0=ALU.add, op1=ALU.add)),
                                 reads=[smb[si], constb], writes=[selbb[qi]])
                            S.op("dve", (lambda e: e.memset(selb[qi][:, vb:vb + 1], 0.0)),
                                 reads=[selbb[qi]], writes=[selbb[qi]])

                        def m0b(qi=qi):
                            S.op("pe", (lambda e: e.transpose(msc2[0:NBLK, :], selb[qi][:], ident[:])),
                                 reads=[selbb[qi], constb], writes=[msc2b])
                            S.op("act", (lambda e: e.activation(out=selbT[qi][:], in_=msc2[0:NBLK, :], func=AF.Copy)),
                                 reads=[msc2b], writes=[selbTb[qi]])

                        def m1(i=i, ei=ei, qi=qi, nk=nk, c0=c0, m_h=m_h, qT=qT, kT=kT, qb=qb, kb=kb):
                            for c in range(-(-nk // 512)):
                                w = min(512, nk - 512 * c)
                                zi = nxt("z", NZ)

                                def zmm(e, zi=zi, c=c, w=w):
                                    e.matmul(zb[zi][:, 0:w], lhsT=qT[:, i * 128:(i + 1) * 128],
                                             rhs=kT[:, 512 * c:512 * c + w], start=True, stop=False)
                                    return e.matmul(zb[zi][:, 0:w], lhsT=selbT[qi][:],
                                                    rhs=ind[:, 512 * c:512 * c + w], start=False, stop=True)
                                S.op("pe", zmm, reads=[qb, kb, selbTb[qi], constb], writes=[zbb[zi]])
                                S.op("dve", (lambda e, zi=zi, c=c, w=w: e.scalar_tensor_tensor(
                                    out=E[ei][:, 512 * c:512 * c + w], in0=b0m[:, c0 + 512 * c:c0 + 512 * c + w],
                                    scalar=-m_h / scale, in1=zb[zi][:, 0:w], op0=ALU.mult, op1=ALU.add)),
                                     reads=[zbb[zi], constb], writes=[Eb[ei]])

                        def m2a(ei=ei, si=si, nk=nk):
                            S.op("dve", (lambda e: e.tensor_reduce(out=sm[si][:, 56:57], in_=E[ei][:, 0:nk],
                                                                   axis=AX.X, op=ALU.max)),
                                 reads=[Eb[ei]], writes=[smb[si]])
                            S.op("dve", (lambda e: e.tensor_scalar(out=sm[si][:, 57:58], in0=sm[si][:, 56:57],
                                                                   scalar1=-scale, scalar2=None, op0=ALU.mult)),
                                 reads=[smb[si]], writes=[smb[si]])

                        def m2b(ei=ei, wi=wi, si=si, nk=nk):
                            S.op("act", (lambda e: e.activation(
                                out=W[wi][:, 0:nk], in_=E[ei][:, 0:nk], func=AF.Exp, bias=sm[si][:, 57:58], scale=scale,
                                accum_out=sm[si][:, 58:59])),
                                 reads=[Eb[ei], smb[si]], writes=[Wb[wi], smb[si]])

                        def m3(i=i, wi=wi, ti_=ti_, si=si, qi=qi, nk=nk, ahi=ahi, V=V, Vb=Vb):
                            S.op("dve", (lambda e: e.reciprocal(out=sm[si][:, 59:60], in_=sm[si][:, 58:59])),
                                 reads=[smb[si]], writes=[smb[si]])
                            transposes(wi, ti_, nk)
                            nt = nk // 128

                            def pv(e):
                                ins = None
                                for j in range(nt):
                                    ins = e.matmul(ob, lhsT=WT[ti_][:, j, :], rhs=V[:, j, :],
                                                   start=(j == 0), stop=(j == nt - 1))
                                return ins
                            S.op("pe", pv, reads=[Vb, WTb[ti_]], writes=[obb])

                        def m4(si=si, qi=qi):
                            S.op("dve", (lambda e: e.tensor_scalar(out=osb[qi][:], in0=ob, scalar1=sm[si][:, 59:60],
                                                                   scalar2=None, op0=ALU.mult)),
                                 reads=[obb, smb[si]], writes=[osbb[qi]])

                        def m4b(qi=qi):
                            S.op("pe", (lambda e: e.transpose(msc, osb[qi][:], ident[:])),
                                 reads=[osbb[qi], constb], writes=[mscb])

                        def m5(i=i, ahi=ahi):
                            S.op("dve", (lambda e: e.tensor_copy(out=ah[ahi][:, i * 128:(i + 1) * 128], in_=msc)),
                                 reads=[mscb], writes=[ahb[ahi]])
                        stg = [(-1, 0, m0p), (0, 0, m0a), (2, 1, m1), (3, 2, m2a), (4, 3, m2b), (5, 4, m3),
                               (6, 3.5, m4), (7, 3.5, m4b), (8, 3.4, m5), (0, 8, m0b)]
                        if i == 0:
                            stg.append((-3, -1, loadfn))
                        if i == QT - 1:
                            stg.append((9, 9, storefn))
                        tiles.append(stg)
                pipeline(tiles)
                S.drain_dmas("sp")
                with nc.Block() as block:
                    S.emit(block)

        pieceb = [Buf("piece%d" % p_) for p_ in range(cfg.NP)]
        phase_ac("A")
        phase_b("sb", True)
        phase_b("moba", False)
        phase_ac("C")
    return nc


def tile_w(W):
    K, N = W.shape
    return np.ascontiguousarray(W.reshape(K // 128, 128, N // 128, 128).transpose(2, 1, 0, 3))


def fm(a):
    T, C = a.shape
    return np.ascontiguousarray(a.T.reshape(C // 128, 128, T).transpose(1, 0, 2))


def consts(cfg, hf):
    t = np.arange(128)[:, None]
    ident = (t == np.arange(128)[None, :]).astype(np.float32)
    strict = (np.arange(128)[None, :] < t).astype(np.float32)
    if hf == 0:
        mask2 = np.concatenate([strict, np.zeros((128, 128), np.float32)], axis=1)
    else:
        mask2 = np.concatenate([np.ones((128, 128), np.float32), strict], axis=1)
    mask2 = (mask2 - 1.0) * np.float32(BIG)
    c = np.arange(cfg.TABW)[None, :]
    d = (t + 128 * hf - (c - cfg.C0)).astype(np.float32)
    b0m = np.where(d >= 0, d, np.float32(1.0e9)).astype(np.float32)
    return ident, np.ascontiguousarray(mask2), b0m


def mask_bias(cfg):
    mbv = np.zeros((cfg.QT, cfg.NBLK), np.float32)
    for i in range(cfg.QT):
        mbv[i, i:] = -1.0e30
    return np.ascontiguousarray(np.broadcast_to(mbv[None], (128, cfg.QT, cfg.NBLK)))


def run(cfg, inp):
    D, SH = cfg.D, cfg.SH
    nc = build_program(cfg)
    f32 = np.float32
    wmap = {"f1g": "ffn1_w_gate", "f1u": "ffn1_w_up", "f1d": "ffn1_w_down", "win": "w_in", "wbm": "w_branch_moba",
            "wbs": "w_branch_sb", "wout": "w_out", "f2g": "ffn2_w_gate", "f2u": "ffn2_w_up", "f2d": "ffn2_w_down",
            "wpg": "w_ple_gate", "wpp": "w_ple_proj"}
    shared = {k: tile_w(np.asarray(inp[v], f32)[0]) for k, v in wmap.items()}
    g = np.stack([np.asarray(inp[n], f32).reshape(-1) for n in
                  ("ffn1_norm", "mix_norm", "ffn2_norm", "ple_norm", "final_norm")])
    shared["gall"] = np.ascontiguousarray(g.reshape(5, cfg.KC, 128).transpose(2, 0, 1))
    shared["cind"] = (np.arange(cfg.SV)[None, :] // 256 == np.arange(cfg.NBLK)[:, None]).astype(np.float32)
    shared["mbias"] = mask_bias(cfg)
    x = np.asarray(inp["x"], f32)
    p = np.asarray(inp["p"], f32)[0]
    nb = cfg.QT
    in_maps = []
    for r in range(cfg.NCORES):
        b, hf = r // 2, r % 2
        m = dict(shared)
        m["cident"], m["cmask2"], m["cb0m"] = consts(cfg, hf)
        m["xT"] = fm(x[b].reshape(nb, 2, 128, D)[:, hf].reshape(SH, D))
        m["pT"] = fm(p[b].reshape(nb, 2, 128, -1)[:, hf].reshape(SH, -1))
        in_maps.append(m)
    res = run_bass_kernel_spmd(nc, in_maps, core_ids=list(range(cfg.NCORES)))
    out = np.empty((cfg.B, 2 * SH, D), f32)
    for r in range(cfg.NCORES):
        b, hf = r // 2, r % 2
        oT = np.asarray(res.results[r]["outT"], f32)
        out[b].reshape(nb, 2, 128, D)[:, hf] = oT.transpose(2, 1, 0).reshape(nb, 128, D)
    return out


def kernel(**inputs):
    return run(FULL, inputs)
```
